# Optimizing a Trainium2 kernel written in Bass

```python
import math
import jax, jax.numpy as jnp
from jax import lax
import numpy as np

D_MODEL = 1024
BATCH = 4
SEQ = 4096
DEPTH = 2

ATTN_HEADS = D_MODEL // 64
ATTN_HEAD_DIM = 64
ROT_DIM = ATTN_HEAD_DIM // 4
ROPE_THETA = 500000.0
MOBA_BLOCK = 256
MOBA_TOP_K = 3
MOBA_Q_CHUNK = 32
MLSTM_HEADS = 4
MLSTM_DV = D_MODEL // MLSTM_HEADS
MLSTM_DQK = MLSTM_DV // 2
MLSTM_CHUNK = 64
MLSTM_PROJ = 2 * MLSTM_HEADS * MLSTM_DQK + 2 * MLSTM_HEADS * MLSTM_DV + 2 * MLSTM_HEADS
MOE_EXPERTS = 32
MOE_TOP_K = 4
MOE_D_FF = D_MODEL
SWIGLU_LIMIT = 7.0
SWIGLU_ALPHA = 1.702
MOE_ROW_BLOCK = 256
N_MIXERS = 2
N_ATTN = (DEPTH + 1) // 2
N_MLSTM = DEPTH // 2
DEEPNORM_ALPHA = (2 * DEPTH) ** 0.25
DEEPNORM_BETA = (8 * DEPTH) ** -0.25
LN_EPS = 1e-5

kernel_name = 'moba_mlstm_interleaved_moe_deepnorm'


def layer_norm(x, g, b):
    xf = x.astype(jnp.float32)
    mu = jnp.mean(xf, axis=-1, keepdims=True)
    var = jnp.mean(jnp.square(xf - mu), axis=-1, keepdims=True)
    y = (xf - mu) * lax.rsqrt(var + LN_EPS)
    return (y * g.astype(jnp.float32) + b.astype(jnp.float32)).astype(x.dtype)


def partial_rope(x, positions):
    inv_freq = ROPE_THETA ** (-jnp.arange(0, ROT_DIM, 2, dtype=jnp.float32) / ROT_DIM)
    ang = positions.astype(jnp.float32)[:, None, :, None] * inv_freq
    cos, sin = jnp.cos(ang), jnp.sin(ang)
    xr = x[..., :ROT_DIM].astype(jnp.float32)
    x1, x2 = xr[..., :ROT_DIM // 2], xr[..., ROT_DIM // 2:]
    rot = jnp.concatenate([x1 * cos - x2 * sin, x2 * cos + x1 * sin], axis=-1)
    return jnp.concatenate([rot.astype(x.dtype), x[..., ROT_DIM:]], axis=-1)


def moba_attention(q, k, v):
    Bb, H, S, Dh = q.shape
    nb = S // MOBA_BLOCK
    kk = min(MOBA_TOP_K, nb)
    nq = S // MOBA_Q_CHUNK
    k_blk = k.reshape(Bb, H, nb, MOBA_BLOCK, Dh)
    v_blk = v.reshape(Bb, H, nb, MOBA_BLOCK, Dh)
    k_mean = jnp.mean(k_blk.astype(jnp.float32), axis=3)
    gate = jnp.einsum('bhsd,bhnd->bhsn', q.astype(jnp.float32), k_mean)
    q_blk = jnp.arange(S) // MOBA_BLOCK
    fully_past = jnp.arange(nb)[None, :] < q_blk[:, None]
    gate = jnp.where(fully_past, gate, -jnp.inf)
    _, sel = lax.top_k(gate, kk)
    valid = sel < q_blk[:, None]
    to_chunks = lambda a: jnp.moveaxis(a.reshape(Bb, H, nq, MOBA_Q_CHUNK, *a.shape[3:]), 2, 0)
    gather = jax.vmap(jax.vmap(lambda blocks, idx: blocks[idx]))
    scale = Dh ** -0.5

    def chunk(args):
        c, q_c, sel_c, valid_c = args
        q_start = c * MOBA_Q_CHUNK
        own = q_start // MOBA_BLOCK
        k_sel = gather(k_blk, sel_c)
        v_sel = gather(v_blk, sel_c)
        k_own = lax.dynamic_index_in_dim(k_blk, own, axis=2, keepdims=False)
        v_own = lax.dynamic_index_in_dim(v_blk, own, axis=2, keepdims=False)
        s_sel = (jnp.einsum('bhqd,bhqjkd->bhqjk', q_c, k_sel) * scale).astype(jnp.float32)
        s_sel = jnp.where(valid_c[..., None], s_sel, -jnp.inf).reshape(Bb, H, MOBA_Q_CHUNK, kk * MOBA_BLOCK)
        s_own = (jnp.einsum('bhqd,bhkd->bhqk', q_c, k_own) * scale).astype(jnp.float32)
        q_pos = q_start + jnp.arange(MOBA_Q_CHUNK)
        k_pos = own * MOBA_BLOCK + jnp.arange(MOBA_BLOCK)
        s_own = jnp.where(k_pos[None, :] <= q_pos[:, None], s_own, -jnp.inf)
        p = jax.nn.softmax(jnp.concatenate([s_sel, s_own], axis=-1), axis=-1).astype(v.dtype)
        p_sel = p[..., :kk * MOBA_BLOCK].reshape(Bb, H, MOBA_Q_CHUNK, kk, MOBA_BLOCK)
        p_own = p[..., kk * MOBA_BLOCK:]
        return (jnp.einsum('bhqjk,bhqjkd->bhqd', p_sel, v_sel)
                + jnp.einsum('bhqk,bhkd->bhqd', p_own, v_own))

    out = lax.map(chunk, (jnp.arange(nq), to_chunks(q), to_chunks(sel), to_chunks(valid)))
    return jnp.moveaxis(out, 0, 2).reshape(Bb, H, S, Dh)


def moba_layer(x, positions, w_qkv, w_o):
    Bb, S, _ = x.shape
    hd = ATTN_HEADS * ATTN_HEAD_DIM
    qkv = x @ w_qkv
    split = lambda a: a.reshape(Bb, S, ATTN_HEADS, ATTN_HEAD_DIM).transpose(0, 2, 1, 3)
    q, k, v = split(qkv[..., :hd]), split(qkv[..., hd:2 * hd]), split(qkv[..., 2 * hd:])
    q, k = partial_rope(q, positions), partial_rope(k, positions)
    s_pad = -(-S // MOBA_BLOCK) * MOBA_BLOCK
    pad = ((0, 0), (0, 0), (0, s_pad - S), (0, 0))
    o = moba_attention(jnp.pad(q, pad), jnp.pad(k, pad), jnp.pad(v, pad))[:, :, :S]
    return o.transpose(0, 2, 1, 3).reshape(Bb, S, hd) @ w_o


def mlstm_chunkwise(q, k, v, i_pre, f_pre):
    Bb, H, S, Dqk = q.shape
    Dv = v.shape[-1]
    nc = S // MLSTM_CHUNK
    f32 = jnp.float32
    to_chunks = lambda a: jnp.moveaxis(a.astype(f32).reshape(Bb, H, nc, MLSTM_CHUNK, *a.shape[3:]), 2, 0)
    log_f = jax.nn.log_sigmoid(f_pre.astype(f32))
    causal = jnp.tril(jnp.ones((MLSTM_CHUNK, MLSTM_CHUNK), dtype=bool))

    def step(carry, xs):
        C, n, m = carry
        q_c, k_c, v_c, i_c, lf_c = xs
        b = jnp.cumsum(lf_c, axis=-1)
        D = jnp.where(causal, b[..., :, None] - b[..., None, :] + i_c[..., None, :], -jnp.inf)
        g = b + m[..., None]
        m_t = jnp.maximum(g, jnp.max(D, axis=-1))
        w_inter = jnp.exp(g - m_t)
        A = jnp.exp(D - m_t[..., None]) * jnp.einsum('bhtd,bhsd->bhts', q_c, k_c)
        num = (w_inter[..., None] * jnp.einsum('bhtd,bhde->bhte', q_c, C)
               + jnp.einsum('bhts,bhse->bhte', A, v_c))
        den = w_inter * jnp.einsum('bhtd,bhd->bht', q_c, n) + jnp.sum(A, axis=-1)
        h = num / jnp.maximum(jnp.abs(den), jnp.exp(-m_t))[..., None]
        m_new = m_t[..., -1]
        decay = jnp.exp(b[..., -1] + m - m_new)
        w_s = jnp.exp(b[..., -1:] - b + i_c - m_new[..., None])
        C_new = decay[..., None, None] * C + jnp.einsum('bhs,bhsd,bhse->bhde', w_s, k_c, v_c)
        n_new = decay[..., None] * n + jnp.einsum('bhs,bhsd->bhd', w_s, k_c)
        return (C_new, n_new, m_new), h

    init = (jnp.zeros((Bb, H, Dqk, Dv), f32), jnp.zeros((Bb, H, Dqk), f32), jnp.zeros((Bb, H), f32))
    _, hs = lax.scan(step, init, (to_chunks(q), to_chunks(k), to_chunks(v), to_chunks(i_pre), to_chunks(log_f)))
    return jnp.moveaxis(hs, 0, 2).reshape(Bb, H, S, Dv)


def mlstm_layer(x, w_in, b_gates, norm_g, w_out):
    Bb, S, _ = x.shape
    qk_w = MLSTM_HEADS * MLSTM_DQK
    v_w = MLSTM_HEADS * MLSTM_DV
    proj = x @ w_in
    heads = lambda a, d: a.reshape(Bb, S, MLSTM_HEADS, d).transpose(0, 2, 1, 3)
    q = heads(proj[..., :qk_w], MLSTM_DQK)
    k = heads(proj[..., qk_w:2 * qk_w], MLSTM_DQK) * (MLSTM_DQK ** -0.5)
    v = heads(proj[..., 2 * qk_w:2 * qk_w + v_w], MLSTM_DV)
    o_pre = proj[..., 2 * qk_w + v_w:2 * qk_w + 2 * v_w]
    gates = (proj[..., 2 * qk_w + 2 * v_w:] + b_gates).transpose(0, 2, 1)
    h = mlstm_chunkwise(q, k, v, gates[:, :MLSTM_HEADS], gates[:, MLSTM_HEADS:])
    mu = jnp.mean(h, axis=-1, keepdims=True)
    var = jnp.mean(jnp.square(h - mu), axis=-1, keepdims=True)
    h = (h - mu) * lax.rsqrt(var + LN_EPS) * norm_g.astype(jnp.float32).reshape(MLSTM_HEADS, 1, MLSTM_DV)
    h = h.transpose(0, 2, 1, 3).reshape(Bb, S, v_w).astype(x.dtype)
    return (jax.nn.sigmoid(o_pre) * h) @ w_out


def clamped_swiglu(h):
    x_glu = jnp.minimum(h[..., ::2], SWIGLU_LIMIT)
    x_lin = jnp.clip(h[..., 1::2], -SWIGLU_LIMIT, SWIGLU_LIMIT)
    return x_glu * jax.nn.sigmoid(SWIGLU_ALPHA * x_glu) * (x_lin + 1.0)


def moe_ffn(x, router_w, router_b, w_gate_up, b_gate_up, w_down, b_down):
    Bb, S, D = x.shape
    xt = x.reshape(-1, D)
    T = xt.shape[0]
    n_assign = T * MOE_TOP_K
    logits = (xt @ router_w + router_b).astype(jnp.float32)
    top_logits, top_e = lax.top_k(logits, MOE_TOP_K)
    gates = jax.nn.softmax(top_logits, axis=-1)
    flat_e = top_e.reshape(-1)
    order = jnp.argsort(flat_e)
    sorted_e = flat_e[order]
    counts = jnp.bincount(flat_e, length=MOE_EXPERTS)
    padded = (counts + MOE_ROW_BLOCK - 1) // MOE_ROW_BLOCK * MOE_ROW_BLOCK
    pad_end = jnp.cumsum(padded)
    pad_start = pad_end - padded
    start = jnp.cumsum(counts) - counts
    dest_sorted = pad_start[sorted_e] + jnp.arange(n_assign, dtype=jnp.int32) - start[sorted_e]
    dest = jnp.zeros((n_assign,), jnp.int32).at[order].set(dest_sorted.astype(jnp.int32))
    n_blocks = -(-(n_assign + MOE_EXPERTS * (MOE_ROW_BLOCK - 1)) // MOE_ROW_BLOCK)
    n_rows = n_blocks * MOE_ROW_BLOCK
    row_token = jnp.zeros((n_rows,), jnp.int32).at[dest].set(jnp.arange(n_assign, dtype=jnp.int32) // MOE_TOP_K)
    block_e = jnp.minimum(jnp.searchsorted(pad_end, jnp.arange(n_blocks) * MOE_ROW_BLOCK, side='right'),
                          MOE_EXPERTS - 1)
    x_rows = xt[row_token].reshape(n_blocks, MOE_ROW_BLOCK, D)

    def expert_block(args):
        xb, e = args
        h = xb @ w_gate_up[e] + b_gate_up[e]
        return clamped_swiglu(h) @ w_down[e] + b_down[e]

    y_rows = lax.map(expert_block, (x_rows, block_e)).reshape(n_rows, D)
    y = y_rows[dest].reshape(T, MOE_TOP_K, D)
    out = jnp.einsum('tkd,tk->td', y, gates.astype(y.dtype))
    return out.reshape(Bb, S, D)


def _normal(key, shape, scale):
    return jax.random.normal(key, shape, jnp.float32) * scale


def setup_inputs(seed: int = 0) -> dict:
    key = jax.random.key(seed)
    ks = jax.random.split(key, 20)
    hd = ATTN_HEADS * ATTN_HEAD_DIM
    v_w = MLSTM_HEADS * MLSTM_DV
    x = _normal(ks[0], (BATCH, SEQ, D_MODEL), 1.0)
    positions = jnp.broadcast_to(jnp.arange(SEQ, dtype=jnp.int32), (BATCH, SEQ))
    attn_w_qkv = _normal(ks[1], (N_ATTN, D_MODEL, 3 * hd), D_MODEL ** -0.5)
    attn_w_o = _normal(ks[2], (N_ATTN, hd, D_MODEL), hd ** -0.5 * DEEPNORM_BETA)
    mlstm_w_in = _normal(ks[3], (N_MLSTM, D_MODEL, MLSTM_PROJ), D_MODEL ** -0.5)
    mlstm_b_gates = jnp.concatenate([
        _normal(ks[4], (N_MLSTM, MLSTM_HEADS), 0.1),
        3.0 + _normal(ks[5], (N_MLSTM, MLSTM_HEADS), 0.1)], axis=-1)
    mlstm_norm_g = 1.0 + _normal(ks[6], (N_MLSTM, v_w), 0.02)
    mlstm_w_out = _normal(ks[7], (N_MLSTM, v_w, D_MODEL), v_w ** -0.5 * DEEPNORM_BETA)
    ln_mix_g = 1.0 + _normal(ks[8], (DEPTH, D_MODEL), 0.02)
    ln_mix_b = _normal(ks[9], (DEPTH, D_MODEL), 0.02)
    ln_ffn_g = 1.0 + _normal(ks[10], (DEPTH, D_MODEL), 0.02)
    ln_ffn_b = _normal(ks[11], (DEPTH, D_MODEL), 0.02)
    router_w = _normal(ks[12], (DEPTH, D_MODEL, MOE_EXPERTS), D_MODEL ** -0.5)
    router_b = _normal(ks[13], (DEPTH, MOE_EXPERTS), 0.01)
    w_gate_up = _normal(ks[14], (DEPTH, MOE_EXPERTS, D_MODEL, 2 * MOE_D_FF), D_MODEL ** -0.5)
    b_gate_up = _normal(ks[15], (DEPTH, MOE_EXPERTS, 2 * MOE_D_FF), 0.01)
    w_down = _normal(ks[16], (DEPTH, MOE_EXPERTS, MOE_D_FF, D_MODEL), MOE_D_FF ** -0.5 * DEEPNORM_BETA)
    b_down = _normal(ks[17], (DEPTH, MOE_EXPERTS, D_MODEL), 0.01)
    return {'x': x, 'positions': positions,
            'attn_w_qkv': attn_w_qkv, 'attn_w_o': attn_w_o,
            'mlstm_w_in': mlstm_w_in, 'mlstm_b_gates': mlstm_b_gates,
            'mlstm_norm_g': mlstm_norm_g, 'mlstm_w_out': mlstm_w_out,
            'ln_mix_g': ln_mix_g, 'ln_mix_b': ln_mix_b, 'ln_ffn_g': ln_ffn_g, 'ln_ffn_b': ln_ffn_b,
            'router_w': router_w, 'router_b': router_b,
            'w_gate_up': w_gate_up, 'b_gate_up': b_gate_up, 'w_down': w_down, 'b_down': b_down}


def reference(x, positions, attn_w_qkv, attn_w_o, mlstm_w_in, mlstm_b_gates, mlstm_norm_g, mlstm_w_out,
              ln_mix_g, ln_mix_b, ln_ffn_g, ln_ffn_b, router_w, router_b,
              w_gate_up, b_gate_up, w_down, b_down):
    for layer in range(DEPTH):
        slot = layer // N_MIXERS
        if layer % N_MIXERS == 0:
            y = moba_layer(x, positions, attn_w_qkv[slot], attn_w_o[slot])
        else:
            y = mlstm_layer(x, mlstm_w_in[slot], mlstm_b_gates[slot], mlstm_norm_g[slot], mlstm_w_out[slot])
        x = layer_norm(DEEPNORM_ALPHA * x + y, ln_mix_g[layer], ln_mix_b[layer])
        f = moe_ffn(x, router_w[layer], router_b[layer], w_gate_up[layer], b_gate_up[layer],
                    w_down[layer], b_down[layer])
        x = layer_norm(DEEPNORM_ALPHA * x + f, ln_ffn_g[layer], ln_ffn_b[layer])
    return x
```

```python
import os, sys, math
from contextlib import ExitStack
import numpy as np
from concourse.bass_utils import run_bass_kernel_spmd

import numpy as np
import concourse.bass as bass
import concourse.mybir as mybir

F32 = mybir.dt.float32
BF16 = mybir.dt.bfloat16
I32 = mybir.dt.int32
AF = mybir.ActivationFunctionType
ALU = mybir.AluOpType
AX = mybir.AxisListType

ENGS = ("pe", "act", "dve", "pool", "sp")


class Prog:
    def __init__(self, nc, stack):
        self.nc = nc
        self.stack = stack
        self.eng = {"pe": nc.tensor, "act": nc.scalar, "dve": nc.vector, "pool": nc.gpsimd, "sp": nc.sync}
        self.sem = {}
        for e in ENGS:
            self.sem[e] = stack.enter_context(nc.semaphore("s_" + e))
        self.cnt = {k: 0 for k in self.sem}
        self.known = {e: {k: 0 for k in self.sem} for e in ENGS}
        self.lane_rr = {}
        self.last_w = {}
        self.readers = {}
        self.ops = []
        self.n_total = 0

    def lane(self, name):
        key = "L:" + name
        if key not in self.sem:
            self.sem[key] = self.stack.enter_context(self.nc.semaphore("sl_" + name))
            self.cnt[key] = 0
            for e in ENGS:
                self.known[e][key] = 0
        return key

    def rr_lane(self, prefix, n):
        i = self.lane_rr.get(prefix, 0)
        self.lane_rr[prefix] = i + 1
        return "%s%d" % (prefix, i % n)

    def op(self, e, fn, r=(), w=(), dma=False, lane=None, inc=None):
        r2, w2 = [], list(w)
        for k in r:
            if isinstance(k, tuple) and k[0] == "ps":
                w2.append(k)
            else:
                r2.append(k)
        w = [(("ps", k[1] % 4) if (isinstance(k, tuple) and k[0] == "ps") else k) for k in w2]
        w = list(dict.fromkeys(w))
        r = r2
        deps = {}
        def add(tok):
            if tok is None:
                return
            s, v = tok
            if deps.get(s, 0) < v:
                deps[s] = v
        for k in r:
            add(self.last_w.get(k))
        for k in w:
            add(self.last_w.get(k))
            for t in self.readers.get(k, ()):
                add(t)
        if dma:
            semname = self.lane(lane if lane is not None else self.rr_lane("misc", 4))
            if self.cnt[semname] > 0:
                add((semname, self.cnt[semname]))
        else:
            semname = e
        step = inc if inc is not None else (16 if dma else 1)
        self.cnt[semname] += step
        tok = (semname, self.cnt[semname])
        for k in r:
            self.readers.setdefault(k, []).append(tok)
        for k in w:
            self.last_w[k] = tok
            self.readers[k] = []
        waits = []
        kn = self.known[e]
        for s, v in deps.items():
            if s == e:
                if e == "pe":
                    continue
                if self.cnt[e] - 1 - v >= 3:
                    continue
            if kn[s] >= v:
                continue
            kn[s] = v
            waits.append((s, v))
        self.ops.append((e, fn, waits, semname, step))
        self.n_total += 1
        return tok

    def barrier(self):
        tot = dict(self.cnt)
        for e in ENGS:
            waits = []
            for s, v in tot.items():
                if v > self.known[e][s] and s != e:
                    self.known[e][s] = v
                    waits.append((s, v))
            if waits:
                self.ops.append((e, None, waits, None, 0))

    def emit(self):
        nc = self.nc
        ops = self.ops
        if not any(o[1] is not None for o in ops):
            return
        self.ops = []
        sem = self.sem
        with nc.Block() as block:
            def make(ename):
                def body(engine):
                    for (e, fn, waits, semname, inc) in ops:
                        if e != ename:
                            continue
                        for s, v in waits:
                            engine.wait_ge(sem[s], v)
                        if fn is not None:
                            ins = fn(engine)
                            ins.then_inc(sem[semname], inc)
                return body
            block.tensor(make("pe"))
            block.scalar(make("act"))
            block.vector(make("dve"))
            block.gpsimd(make("pool"))
            block.sync(make("sp"))

from contextlib import ExitStack

ALPHA = 4 ** 0.25
LN_EPS = 1e-5
NTT = 16

_SBT_N = [0]


def sbt(nc, st, name, shape, dt, side=None):
    _SBT_N[0] += 1
    nm = "%s_%d" % (name, _SBT_N[0])
    if side is None:
        return st.enter_context(nc.sbuf_tensor(nm, shape, dt))
    return st.enter_context(nc.sbuf_tensor(nm, shape, dt, side=side))


def layer_norm_out(P, nc, st, X, lng, lnb, out_dram, tag, xdst=None):
    G = sbt(nc, st, tag + "G", [128, 1024], F32)
    B = sbt(nc, st, tag + "B", [128, 1024], F32)
    junk = sbt(nc, st, tag + "junk", [128, 1024], F32)
    stat = sbt(nc, st, tag + "stat", [128, NTT, 8], F32)
    P.op("sp", lambda e: e.dma_start(out=G[:], in_=lng.partition_broadcast(128)), w=[tag + "G"], dma=True)
    P.op("sp", lambda e: e.dma_start(out=B[:], in_=lnb.partition_broadcast(128)), w=[tag + "B"], dma=True)
    for tt in range(NTT):
        xk = ("X", tt)
        sk = (tag + "stat", tt)
        P.op("act", lambda e, tt=tt: e.activation(out=junk[:], in_=X[:, tt, :], func=AF.Identity, accum_out=stat[:, tt, 0:1]), r=[xk], w=[tag + "junk", sk])
        P.op("act", lambda e, tt=tt: e.activation(out=junk[:], in_=X[:, tt, :], func=AF.Square, accum_out=stat[:, tt, 1:2]), r=[xk], w=[tag + "junk", sk])
        P.op("dve", lambda e, tt=tt: e.tensor_scalar(stat[:, tt, 2:4], stat[:, tt, 0:2], 1.0 / 1024, None, ALU.mult), r=[sk], w=[sk])
        P.op("dve", lambda e, tt=tt: e.tensor_tensor(stat[:, tt, 4:5], stat[:, tt, 2:3], stat[:, tt, 2:3], ALU.mult), r=[sk], w=[sk])
        P.op("dve", lambda e, tt=tt: e.tensor_tensor(stat[:, tt, 5:6], stat[:, tt, 3:4], stat[:, tt, 4:5], ALU.subtract), r=[sk], w=[sk])
        P.op("dve", lambda e, tt=tt: e.tensor_scalar(stat[:, tt, 5:6], stat[:, tt, 5:6], LN_EPS, None, ALU.add), r=[sk], w=[sk])
        P.op("act", lambda e, tt=tt: e.sqrt(stat[:, tt, 7:8], stat[:, tt, 5:6]), r=[sk], w=[sk])
        P.op("dve", lambda e, tt=tt: e.reciprocal(stat[:, tt, 6:7], stat[:, tt, 7:8]), r=[sk], w=[sk])
        P.op("dve", lambda e, tt=tt: e.tensor_scalar(X[:, tt, :], X[:, tt, :], stat[:, tt, 2:3], stat[:, tt, 6:7], ALU.subtract, ALU.mult), r=[sk, xk], w=[xk])
        P.op("dve", lambda e, tt=tt: e.tensor_tensor(X[:, tt, :], X[:, tt, :], G[:], ALU.mult), r=[xk, tag + "G"], w=[xk])
        P.op("dve", lambda e, tt=tt: e.tensor_tensor(X[:, tt, :], X[:, tt, :], B[:], ALU.add), r=[xk, tag + "B"], w=[xk])
        if out_dram is not None:
            P.op("sp", lambda e, tt=tt: e.dma_start(out=out_dram[tt * 128:(tt + 1) * 128, :], in_=X[:, tt, :]), r=[xk], w=[("out", tt)], dma=True, lane=P.rr_lane("out", 4))


import os
STAGES = os.environ.get('STAGES', 'ABC')
LEVEL = int(os.environ.get('LEVEL', '9'))
def moe_stage(P, nc, X, PS, ident, rw, rb, wgu, bgu, wd, bd, lng, lnb, out_dram, E=32, NGU=12, ND=8):
    with ExitStack() as st:
        xT = sbt(nc, st, "xT", [128, 8, 2048], BF16)
        gates = sbt(nc, st, "gates", [128, NTT, E], F32)
        bgT = sbt(nc, st, "bgT", [128, 16, E], F32)
        bgs = sbt(nc, st, "bgs", [128, 8, E], F32)
        WGU = sbt(nc, st, "WGU", [128, NGU, 8, 256], BF16)
        WD = sbt(nc, st, "WD", [128, ND, 1024], BF16)

        NEXP = int(os.environ.get('NEXP', str(E)))
        n_gu = NEXP * 8
        gu_dma_next = [0]
        wd_dma_next = [0]

        def dma_gu(g):
            e_, fc = divmod(g, 8)
            s = g % NGU
            P.op("pool", lambda e: e.dma_start(out=WGU[:, s, :, :], in_=wgu[e_, :, fc * 256:(fc + 1) * 256].rearrange("(k p) n -> p k n", p=128)),
                 w=[("wgu", s)], dma=True, lane="wgu%d" % s)

        def dma_wd(g):
            e_, fc = divmod(g, 8)
            s = g % ND
            P.op("pool", lambda e: e.dma_start(out=WD[:, s, :], in_=wd[e_, fc * 128:(fc + 1) * 128, :]), w=[("wd", s)], dma=True, lane="wd%d" % s)

        if 'B' not in STAGES: n_gu = 0
        for g in range(min(NGU, n_gu)):
            dma_gu(g)
        gu_dma_next[0] = min(NGU, n_gu)
        for g in range(min(ND, n_gu)):
            dma_wd(g)
        wd_dma_next[0] = min(ND, n_gu)

        with ExitStack() as sa:
            bgu_raw = sbt(nc, sa, "bgu_raw", [E, 2048], F32)
            bd_sb = sbt(nc, sa, "bd_sb", [E, 1024], F32)
            rw_sb = sbt(nc, sa, "rw_sb", [128, 8, E], F32)
            rb_sb = sbt(nc, sa, "rb_sb", [128, E], F32)
            xTf = sbt(nc, sa, "xTf", [128, 8, 128], F32)
            lg = sbt(nc, sa, "lg", [128, E], F32)
            ex = sbt(nc, sa, "ex", [128, E], F32)
            mask = sbt(nc, sa, "mask", [128, E], F32)
            top8 = sbt(nc, sa, "top8", [128, 8], F32)
            sm = sbt(nc, sa, "sm", [128, 4], F32)
            gT = sbt(nc, sa, "gT", [E, 128], F32)
            if not os.environ.get('SKIPB'):
                P.op("sp", lambda e: e.dma_start(out=bgu_raw[:], in_=bgu), w=["bgu_raw"], dma=True)
                P.op("sp", lambda e: e.dma_start(out=bd_sb[:], in_=bd), w=["bd_sb"], dma=True)
                P.op("sp", lambda e: e.dma_start(out=rw_sb[:], in_=rw.rearrange("(k p) n -> p k n", p=128)), w=["rw_sb"], dma=True)
                P.op("sp", lambda e: e.dma_start(out=rb_sb[:], in_=rb.partition_broadcast(128)), w=["rb_sb"], dma=True)
                pb = PS[:, 6, 0:16 * E].rearrange("p (a e) -> p a e", e=E)
                for fc in range(8):
                    for j in range(2):
                        P.op("pe", lambda e, fc=fc, j=j: e.transpose(pb[:, fc * 2 + j, :], bgu_raw[:, fc * 256 + j:(fc + 1) * 256:2], ident[0:E, 0:E]),
                             r=["bgu_raw", "ident"], w=[("ps", 6)])
                P.op("act", lambda e: e.copy(bgT[:], pb), r=[("ps", 6)], w=["bgT"])
                bgT_g = bgT[:].rearrange("p (f j) e -> p f j e", j=2)[:, :, 0, :]
                P.op("dve", lambda e: e.tensor_scalar(bgs[:], bgT_g, 1.702, None, ALU.mult), r=["bgT"], w=["bgs"])
            ptr = PS[:, 0:2, :].rearrange("p b (k n) -> p (b k) n", n=128)
            plg = PS[:, 2, 0:E]
            pgT = PS[0:E, 3, 0:128]
            pbd = PS[:, 4:6, :]
            for tt in range(NTT):
                xk = ("X", tt)
                for k in range(8):
                    P.op("pe", lambda e, tt=tt, k=k: e.transpose(ptr[:, k, :], X[:, tt, k * 128:(k + 1) * 128], ident[:]),
                         r=[xk, "ident"], w=[("ps", k // 4)])
                P.op("act", lambda e, tt=tt: e.copy(xT[:, :, tt * 128:(tt + 1) * 128], ptr), r=[("ps", 0), ("ps", 1)], w=[("xT", tt // 4)])
                P.op("act", lambda e: e.copy(xTf[:], ptr), r=[("ps", 0), ("ps", 1)], w=["xTf"])
                if LEVEL < 2: continue
                for k in range(8):
                    P.op("pe", lambda e, k=k: e.matmul(plg, xTf[:, k, :], rw_sb[:, k, :], start=(k == 0), stop=(k == 7)),
                         r=["xTf", "rw_sb"], w=[("ps", 2)])
                P.op("dve", lambda e: e.tensor_tensor(lg[:], plg, rb_sb[:], ALU.add), r=[("ps", 2), "rb_sb"], w=["lg"])
                P.op("dve", lambda e: e.max(out=top8[:], in_=lg[:]), r=["lg"], w=["top8"])
                if LEVEL < 3: continue
                P.op("dve", lambda e: e.tensor_scalar(mask[:], lg[:], top8[:, 3:4], None, ALU.is_ge), r=["lg", "top8"], w=["mask"])
                P.op("dve", lambda e: e.tensor_scalar(sm[:, 0:1], top8[:, 0:1], -1.0, None, ALU.mult), r=["top8"], w=["sm0"])
                P.op("act", lambda e: e.activation(out=ex[:], in_=lg[:], func=AF.Exp, bias=sm[:, 0:1], scale=1.0), r=["lg", "sm0"], w=["ex"])
                P.op("dve", lambda e: e.tensor_tensor(ex[:], ex[:], mask[:], ALU.mult), r=["ex", "mask"], w=["ex"])
                P.op("dve", lambda e: e.reduce_sum(out=sm[:, 1:2], in_=ex[:], axis=AX.X), r=["ex"], w=["sm1"])
                P.op("dve", lambda e: e.reciprocal(sm[:, 2:3], sm[:, 1:2]), r=["sm1"], w=["sm2"])
                P.op("dve", lambda e, tt=tt: e.tensor_scalar(gates[:, tt, :], ex[:], sm[:, 2:3], None, ALU.mult), r=["ex", "sm2"], w=[("gates", tt)])
                if LEVEL < 4: continue
                P.op("pe", lambda e, tt=tt: e.transpose(pgT, gates[:, tt, :], ident[:]), r=[("gates", tt), "ident"], w=[("ps", 3)])
                P.op("act", lambda e: e.copy(gT[:], pgT), r=[("ps", 3)], w=["gT"])
                for h in range(2):
                    P.op("pe", lambda e, h=h: e.matmul(pbd[:, h, :], gT[:], bd_sb[:, h * 512:(h + 1) * 512], start=True, stop=True),
                         r=["gT", "bd_sb"], w=[("ps", 4 + h)])
                P.op("dve", lambda e, tt=tt: e.scalar_tensor_tensor(out=X[:, tt, :], in0=X[:, tt, :], scalar=ALPHA, in1=pbd.rearrange("p b n -> p (b n)"), op0=ALU.mult, op1=ALU.add),
                     r=[xk, ("ps", 4), ("ps", 5)], w=[xk])
            P.barrier()
            P.emit()

        with ExitStack() as sb_:
            AS = int(os.environ.get('ACTS', '1'))
            actT = sbt(nc, sb_, "actT", [128, 2, 8, 512 * AS], BF16)
            tg_ = sbt(nc, sb_, "tg_", [128, 2, 512], F32)
            ts_ = sbt(nc, sb_, "ts_", [128, 2, 512], F32)
            tl_ = sbt(nc, sb_, "tl_", [128, 2, 512], F32)
            cnt = [0]

            def GU(i):
                e_, tg = divmod(i, NTT // 4)
                par = i % 2
                for fc in range(8):
                    g = e_ * 8 + fc
                    s = g % NGU
                    q = cnt[0] % 2
                    cnt[0] += 1
                    bg_, bl_ = 0 + q, 2 + q
                    for j, bank in ((0, bg_), (1, bl_)):
                        for k in range(8):
                            P.op("pe", lambda e, s=s, k=k, j=j, bank=bank, tg=tg: e.matmul(PS[:, bank, :], WGU[:, s, k, j:256:2], xT[:, k, tg * 512:(tg + 1) * 512], start=(k == 0), stop=(k == 7)),
                                 r=[("wgu", s), ("xT", tg)], w=[("ps", bank)])
                    if tg == NTT // 4 - 1 and gu_dma_next[0] < n_gu:
                        dma_gu(gu_dma_next[0])
                        gu_dma_next[0] += 1
                    if os.environ.get('NOSW'): continue
                    if os.environ.get('SWONLY') != 'dve': P.op("act", lambda e, q=q, bank=bg_, fc=fc, e_=e_: e.activation(out=ts_[:, q, :], in_=PS[:, bank, :], func=AF.Sigmoid, bias=bgs[:, fc, e_:e_ + 1], scale=1.702),
                         r=[("ps", bg_), "bgs"], w=[("ts", q)])
                    if os.environ.get('SWONLY') not in ('act', 'dvelin'): P.op("dve", lambda e, q=q, bank=bg_, fc=fc, e_=e_: e.tensor_scalar(tg_[:, q, :], PS[:, bank, :], bgT[:, fc * 2, e_:e_ + 1], 7.0, ALU.add, ALU.min),
                         r=[("ps", bg_), "bgT"], w=[("tg", q)])
                    if os.environ.get('SWONLY') != 'act': P.op("dve", lambda e, q=q, bank=bl_, fc=fc, e_=e_: e.tensor_scalar(tl_[:, q, :], PS[:, bank, :], bgT[:, fc * 2 + 1, e_:e_ + 1], 7.0, ALU.add, ALU.min),
                         r=[("ps", bl_), "bgT"], w=[("tl", q)])
                    if os.environ.get('SWONLY') not in ('act', 'dvepsum'): P.op("dve", lambda e, q=q: e.tensor_scalar(tl_[:, q, :], tl_[:, q, :], -7.0, 1.0, ALU.max, ALU.add), r=[("tl", q)], w=[("tl", q)])
                    if os.environ.get('SWONLY') not in ('act', 'dvepsum'): P.op("dve", lambda e, q=q: e.tensor_tensor(tg_[:, q, :], tg_[:, q, :], ts_[:, q, :], ALU.mult), r=[("tg", q), ("ts", q)], w=[("tg", q)])
                    if os.environ.get('SWONLY') not in ('act', 'dvepsum'): P.op("dve", lambda e, q=q, par=par, fc=fc: e.tensor_tensor(actT[:, par, fc, 0:512 * AS:AS], tg_[:, q, :], tl_[:, q, :], ALU.mult),
                         r=[("tg", q), ("tl", q)], w=[("actT", par, fc)])

            def DP(i):
                e_, tg = divmod(i, NTT // 4)
                par = i % 2
                for tq in range(4):
                    tt = tg * 4 + tq
                    b0 = 4 + 2 * (tt % 2)
                    if os.environ.get('DPBANK'): b0 = int(os.environ['DPBANK'])
                    for h in range(2):
                        for fc in range(8):
                            g = e_ * 8 + fc
                            s = g % ND
                            P.op("pe", lambda e, h=h, fc=fc, s=s, par=par, tq=tq, b0=b0: e.matmul(PS[:, b0 + h, :], (xT[:, fc, tq * 128:(tq + 1) * 128] if os.environ.get('DPXT') else actT[:, par, fc, tq * 128 * AS:(tq + 1) * 128 * AS:AS]), WD[:, s, h * 512:(h + 1) * 512], start=(fc == 0), stop=(fc == 7)),
                                 r=([("wd", s)] if os.environ.get('DPXT') == '2' else [("actT", par, fc), ("wd", s)]), w=[("ps", b0 + h)])
                    if tt == NTT - 1:
                        for fc in range(8):
                            if wd_dma_next[0] < n_gu:
                                dma_wd(wd_dma_next[0])
                                wd_dma_next[0] += 1
                    if os.environ.get('NOSTT'): continue
                    P.op("dve", lambda e, tt=tt, e_=e_, b0=b0: e.scalar_tensor_tensor(out=X[:, tt, :], in0=PS[:, b0:b0 + 2, :].rearrange("p b n -> p (b n)"), scalar=gates[:, tt, e_:e_ + 1], in1=X[:, tt, :], op0=ALU.mult, op1=ALU.add),
                         r=[("ps", b0), ("ps", b0 + 1), ("gates", tt), ("X", tt)], w=[("X", tt)])

            n_items = NEXP * (NTT // 4) if 'B' in STAGES else 0
            for i in range(n_items + 1):
                if i < n_items:
                    GU(i)
                if i >= 1 and not os.environ.get('NODP') and (i - 1) < int(os.environ.get('DPMAX', '99999')):
                    DP(i - 1)
            P.barrier()
            P.emit()

        with ExitStack() as sc:
            if 'C' in STAGES:
                layer_norm_out(P, nc, sc, X, lng, lnb, out_dram, "lnf")
            else:
                for tt in range(NTT):
                    P.op("sp", lambda e, tt=tt: e.dma_start(out=out_dram[tt * 128:(tt + 1) * 128, :], in_=X[:, tt, :]), r=[("X", tt)], w=[("out", tt)], dma=True, lane=P.rr_lane("out", 4))
            P.barrier()
            P.emit()


def build_moe_prog(E=32):
    nc = bass.Bass("TRN2", target_bir_lowering=False)
    dt = lambda name, shape, kind="ExternalInput": nc.dram_tensor(name, shape, F32, kind=kind).ap()
    x = dt("x", [NTT * 128, 1024])
    identd = dt("ident", [128, 128])
    rw = dt("rw", [1024, E]); rb = dt("rb", [1, E])
    EE = E if 'B' in STAGES else 1
    wgu = dt("wgu", [EE, 1024, 2048]); bgu = dt("bgu", [E, 2048])
    wd = dt("wd", [EE, 1024, 1024]); bd = dt("bd", [E, 1024])
    lng = dt("lng", [1, 1024]); lnb = dt("lnb", [1, 1024])
    y = dt("y", [NTT * 128, 1024], "ExternalOutput")
    with ExitStack() as st:
        P = Prog(nc, st)
        X = sbt(nc, st, "X", [128, NTT, 1024], F32)
        ident = sbt(nc, st, "identsb", [128, 128], F32)
        PS = st.enter_context(nc.psum_tensor("PS", [128, 8, 512], F32))
        P.op("sp", lambda e: e.dma_start(out=ident[:], in_=identd), w=["ident"], dma=True)
        for tt in range(NTT):
            P.op("sp", lambda e, tt=tt: e.dma_start(out=X[:, tt, :], in_=x[tt * 128:(tt + 1) * 128, :]), w=[("X", tt)], dma=True, lane=P.rr_lane("in", 8))
        moe_stage(P, nc, X, PS, ident, rw, rb, wgu, bgu, wd, bd, lng, lnb, y, E=E)
        print("total ops", P.n_total)
    return nc
from contextlib import ExitStack

BIG = 30000.0
PI = math.pi


def attn_stage(P, nc, X, PS, ident, xkv, pos, wqkv, wo, ropec, pastmask, causal, lng, lnb, out_dram, NTK=32, NTO=16, NH=16, xstack=None):
    TK = NTK * 128
    TO = NTO * 128
    NB = NTK // 2
    NBP = (NTK - NTO) // 2
    own0 = TK - TO
    PSb = PS[:, 2, :].bitcast(BF16)
    with ExitStack() as st:
        O = sbt(nc, st, "a_O", [128, NTO, 1024], BF16)
        rc = sbt(nc, st, "a_rc", [128, 8], F32)
        pm = sbt(nc, st, "a_pm", [128, NTO, NB], F32)
        cm = sbt(nc, st, "a_cm", [128, 2, 256], F32)
        identb = sbt(nc, st, "a_identb", [128, 128], BF16)
        s1 = ExitStack()
        xT = sbt(nc, s1, "a_xT", [128, 8, TK], BF16)
        CS = sbt(nc, s1, "a_CS", [64, 2, TK], BF16)
        P.op("sp", lambda e: e.dma_start(out=rc[:], in_=ropec), w=["rc"], dma=True)
        P.op("sp", lambda e: e.dma_start(out=pm[:], in_=pastmask.rearrange("p (t n) -> p t n", n=NB)), w=["pm"], dma=True)
        P.op("sp", lambda e: e.dma_start(out=cm[:], in_=causal.rearrange("p (t n) -> p t n", n=256)), w=["cm"], dma=True)
        P.op("dve", lambda e: e.tensor_copy(identb[:], ident[:]), r=["ident"], w=["identb"])
        if NH < 16:
            P.op("pool", lambda e: e.memset(O[:], 0.0), w=[("O", t) for t in range(NTO)])
        with ExitStack() as sa:
            RC = min(1024, TK)
            posi = sbt(nc, sa, "a_posi", [64, RC], I32)
            ang = sbt(nc, sa, "a_ang", [64, RC], F32)
            m1 = sbt(nc, sa, "a_m1", [64, RC], F32)
            xtmp = sbt(nc, sa, "a_xtmp", [128, 2, 1024], F32)
            ti = posi
            def fold(dst):
                P.op("dve", lambda e: e.tensor_scalar(m1[:], dst[:], PI, -2 * PI, ALU.is_gt, ALU.mult), r=["ang"], w=["m1"])
                P.op("dve", lambda e: e.tensor_tensor(dst[:], dst[:], m1[:], ALU.add), r=["ang", "m1"], w=["ang"])
                P.op("dve", lambda e: e.tensor_scalar(m1[:], dst[:], -PI, 2 * PI, ALU.is_lt, ALU.mult), r=["ang"], w=["m1"])
                P.op("dve", lambda e: e.tensor_tensor(dst[:], dst[:], m1[:], ALU.add), r=["ang", "m1"], w=["ang"])
            for rcn in range(TK // RC):
                cs_ = slice(rcn * RC, (rcn + 1) * RC)
                P.op("sp", lambda e, cs_=cs_: e.dma_start(out=posi[:], in_=pos[:, cs_].partition_broadcast(64)), w=["posi"], dma=True)
                P.op("dve", lambda e: e.tensor_copy(ang[:], posi[:]), r=["posi"], w=["ang"])
                P.op("dve", lambda e: e.tensor_scalar(ang[:], ang[:], rc[0:64, 0:1], None, ALU.mult), r=["ang", "rc"], w=["ang"])
                P.op("dve", lambda e: e.tensor_scalar(m1[:], ang[:], 1.0 / (2 * PI), None, ALU.mult), r=["ang"], w=["m1"])
                P.op("dve", lambda e: e.tensor_copy(ti[:], m1[:]), r=["m1", "ang"], w=["posi"])
                P.op("dve", lambda e: e.tensor_copy(m1[:], ti[:]), r=["posi"], w=["m1"])
                P.op("dve", lambda e: e.scalar_tensor_tensor(out=ang[:], in0=m1[:], scalar=-2 * PI, in1=ang[:], op0=ALU.mult, op1=ALU.add), r=["m1", "ang"], w=["ang"])
                fold(ang)
                P.op("act", lambda e, cs_=cs_: e.activation(out=CS[:, 1, cs_], in_=ang[:], func=AF.Sin), r=["ang"], w=["CS1"])
                P.op("dve", lambda e, cs_=cs_: e.tensor_scalar(CS[:, 1, cs_], CS[:, 1, cs_], rc[0:64, 1:2], None, ALU.mult), r=["CS1", "rc"], w=["CS1"])
                P.op("dve", lambda e: e.tensor_scalar(ang[:], ang[:], 0.5 * PI, None, ALU.add), r=["ang", "CS1"], w=["ang"])
                fold(ang)
                P.op("act", lambda e, cs_=cs_: e.activation(out=CS[:, 0, cs_], in_=ang[:], func=AF.Sin), r=["ang"], w=["CS0"])
                P.op("dve", lambda e, cs_=cs_: e.tensor_scalar(CS[:, 0, cs_], CS[:, 0, cs_], rc[0:64, 2:3], rc[0:64, 3:4], ALU.mult, ALU.add), r=["CS0", "rc"], w=["CS0"])
            ptr = PS[:, 0:2, :].rearrange("p b (k n) -> p (b k) n", n=128)
            for tt in range(NTK):
                q = tt % 2
                P.op("sp", lambda e, tt=tt, q=q: e.dma_start(out=xtmp[:, q, :], in_=xkv[tt * 128:(tt + 1) * 128, :]), w=[("xtmp", q)], dma=True, lane="ax%d" % q)
                for k in range(8):
                    P.op("pe", lambda e, q=q, k=k: e.transpose(ptr[:, k, :], xtmp[:, q, k * 128:(k + 1) * 128], ident[:]), r=[("xtmp", q), "ident"], w=[("ps", k // 4)])
                P.op("act", lambda e, tt=tt: e.copy(xT[:, :, tt * 128:(tt + 1) * 128], ptr), r=[("ps", 0), ("ps", 1)], w=[("xT", tt)])
            P.barrier(); P.emit()
        xTk = [("xT", tt) for tt in range(NTK)]
        with ExitStack() as sh:
            Wh = sbt(nc, sh, "a_Wh", [128, 8, 192], BF16)
            Wr = sbt(nc, sh, "a_Wr", [128, 8, 128], BF16)
            QT = sbt(nc, sh, "a_QT", [64, 2, TO], BF16)
            KT = sbt(nc, sh, "a_KT", [64, 2, TK], BF16)
            V = sbt(nc, sh, "a_V", [128, 2, NTK, 64], BF16)
            t1 = sbt(nc, sh, "a_t1", [64, 2, 512], F32)
            t2 = sbt(nc, sh, "a_t2", [64, 2, 512], F32)
            ksum = sbt(nc, sh, "a_ksum", [64, NB], F32)
            kmb = sbt(nc, sh, "a_kmb", [64, NB], BF16)
            gm = sbt(nc, sh, "a_gm", [128, NTO, NB], F32)
            top8 = sbt(nc, sh, "a_top8", [128, NTO, 8], F32)
            thr = sbt(nc, sh, "a_thr", [128, NTO], F32)
            bias2 = sbt(nc, sh, "a_bias", [128, 2, NTO, NB], F32)
            Qsq = sbt(nc, sh, "a_Qsq", [64, TO], BF16)
            Ksq = sbt(nc, sh, "a_Ksq", [64, TK], BF16)
            onesc = sbt(nc, sh, "a_onesc", [128, 128], F32)
            onescb = sbt(nc, sh, "a_onescb", [64, 2], BF16)
            kmx = sbt(nc, sh, "a_kmx", [1, 16], F32)
            kb = sbt(nc, sh, "a_kb", [128, 2], F32)
            Mq2 = sbt(nc, sh, "a_Mq", [128, 2, 2, NTO], F32)
            dparts = sbt(nc, sh, "a_dparts", [128, NTO, NB + 1], F32)
            otmp = sbt(nc, sh, "a_otmp", [128, 2, 256], F32)
            Pb = sbt(nc, sh, "a_Pb", [128, 2, TK], BF16)
            PT = sbt(nc, sh, "a_PT", [128, 2, 8, 128], BF16)
            sm = sbt(nc, sh, "a_sm", [128, NTO, 4], F32)
            P.op("pool", lambda e: e.memset(Wr[:], 0.0), w=["Wr"])
            P.op("dve", lambda e: e.memset(onesc[:], 1.0), w=["onesc"])
            P.op("dve", lambda e: e.memset(onescb[:], 1.0), w=["onescb"])
            ptc = [0]
            evc = [0]
            pjc = [0]
            def prelude(h):
                hp = h % 2
                QTh = QT[:, hp, :]; KTh = KT[:, hp, :]; Vh = V[:, hp, :, :]
                bias = bias2[:, hp, :, :]; Mq = Mq2[:, hp, :, :]
                for j, c0 in enumerate((h * 64, 1024 + h * 64, 2048 + h * 64)):
                    P.op("pool", lambda e, j=j, c0=c0: e.dma_start(out=Wh[:, :, j * 64:(j + 1) * 64], in_=wqkv[:, c0:c0 + 64].rearrange("(k p) n -> p k n", p=128)),
                         w=[("Wh", j)], dma=True, lane="wh%d" % j)
                for j in range(2):
                    P.op("act", lambda e, j=j: e.copy(Wr[:, :, j * 64:j * 64 + 8], Wh[:, :, j * 64 + 8:j * 64 + 16]), r=[("Wh", j)], w=["Wr"])
                    P.op("act", lambda e, j=j: e.copy(Wr[:, :, j * 64 + 8:j * 64 + 16], Wh[:, :, j * 64:j * 64 + 8]), r=[("Wh", j)], w=["Wr"])
                for (j, dst, tok0, nch, scl, dk) in ((0, QTh, own0, TO // 512, 0.125, "QT"), (1, KTh, 0, TK // 512, 1.0, "KT")):
                    for c in range(nch):
                        a0 = tok0 + c * 512
                        pj = pjc[0] % 2
                        pjc[0] += 1
                        b0_, b1_ = 2 * pj, 2 * pj + 1
                        for (bank, Wt) in ((b0_, Wh), (b1_, Wr)):
                            for k in range(8):
                                P.op("pe", lambda e, bank=bank, Wt=Wt, k=k, j=j, a0=a0: e.matmul(PS[0:64, bank, :], Wt[:, k, j * 64:(j + 1) * 64], xT[:, k, a0:a0 + 512], start=(k == 0), stop=(k == 7)),
                                     r=[("Wh", j), "Wr"] + xTk[a0 // 128:a0 // 128 + 4], w=[("ps", bank)])
                        P.op("dve", lambda e, a0=a0, pj=pj, b0_=b0_: e.tensor_tensor(t1[:, pj, :], PS[0:64, b0_, :], CS[:, 0, a0:a0 + 512], ALU.mult), r=[("ps", b0_), "CS0"], w=[("t1", pj)])
                        P.op("dve", lambda e, a0=a0, scl=scl, pj=pj, b1_=b1_: e.scalar_tensor_tensor(out=t2[:, pj, :], in0=PS[0:64, b1_, :], scalar=scl, in1=CS[:, 1, a0:a0 + 512], op0=ALU.mult, op1=ALU.mult), r=[("ps", b1_), "CS1"], w=[("t2", pj)])
                        P.op("dve", lambda e, dst=dst, c=c, scl=scl, pj=pj: e.scalar_tensor_tensor(out=dst[:, c * 512:(c + 1) * 512], in0=t1[:, pj, :], scalar=scl, in1=t2[:, pj, :], op0=ALU.mult, op1=ALU.add), r=[("t1", pj), ("t2", pj)], w=[(dk, hp, c)])
                QTk = [("QT", hp, c) for c in range(TO // 512)]
                KTk = [("KT", hp, c) for c in range(TK // 512)]
                for g8 in range(NTK // 8):
                    pv = PS[:, 2, :].rearrange("p (t d) -> p t d", d=64)
                    for i in range(8):
                        tt = g8 * 8 + i
                        for k in range(8):
                            P.op("pe", lambda e, i=i, tt=tt, k=k: e.matmul(pv[:, i, :], xT[:, k, tt * 128:(tt + 1) * 128], Wh[:, k, 128:192], start=(k == 0), stop=(k == 7)),
                                 r=[("Wh", 2), ("xT", tt)], w=[("ps", 2)])
                    P.op("act", lambda e, g8=g8, Vh=Vh: e.copy(Vh[:, g8 * 8:(g8 + 1) * 8, :], pv), r=[("ps", 2)], w=[("V", hp, g8)])
                Vk = [("V", g8) for g8 in range(NTK // 8)]
                P.op("dve", lambda e, KTh=KTh: e.tensor_reduce(out=ksum[:], in_=KTh.rearrange("p (n k) -> p n k", k=256), axis=AX.X, op=ALU.add), r=KTk, w=["ksum"])
                P.op("dve", lambda e: e.tensor_copy(kmb[:], ksum[:]), r=["ksum"], w=["kmb"])
                pg = PS[:, 3, 0:NTO * NB].rearrange("p (t n) -> p t n", n=NB)
                for t in range(NTO):
                    P.op("pe", lambda e, t=t, QTh=QTh: e.matmul(pg[:, t, :], QTh[:, t * 128:(t + 1) * 128], kmb[:], start=True, stop=True), r=[("QT", hp, t // 4), "kmb"], w=[("ps", 3)])
                P.op("dve", lambda e: e.tensor_tensor(gm[:], pg, pm[:], ALU.add), r=[("ps", 3), "pm"], w=["gm"])
                for t in range(NTO):
                    P.op("dve", lambda e, t=t: e.max(out=top8[:, t, :], in_=gm[:, t, :]), r=["gm"], w=["top8"])
                P.op("dve", lambda e: e.tensor_scalar(thr[:], top8[:, :, 2], -1e29, None, ALU.max), r=["top8"], w=["thr"])
                for t in range(NTO):
                    P.op("dve", lambda e, t=t: e.tensor_scalar(bias[:, t, :], gm[:, t, :], thr[:, t:t + 1], 1.0, ALU.is_ge, ALU.subtract), r=["gm", "thr"], w=[("bias", hp)])
                P.op("dve", lambda e: e.tensor_scalar(bias[:], bias[:], BIG, None, ALU.mult), r=[("bias", hp)], w=[("bias", hp)])
                P.op("dve", lambda e, QTh=QTh: e.tensor_tensor(Qsq[:], QTh, QTh, ALU.mult), r=QTk, w=["Qsq"])
                P.op("dve", lambda e, KTh=KTh: e.tensor_tensor(Ksq[:], KTh, KTh, ALU.mult), r=KTk, w=["Ksq"])
                pn = PS[:, 3, 256:256 + NTO]
                for t in range(NTO):
                    P.op("pe", lambda e, t=t: e.matmul(PS[:, 3, 256 + t:257 + t], Qsq[:, t * 128:(t + 1) * 128], onescb[:, 0:1], start=True, stop=True), r=["Qsq", "onescb"], w=[("ps", 3)])
                for c in range(TK // 512):
                    P.op("pe", lambda e, c=c: e.matmul(PS[0:1, c % 2, :], onescb[:, 0:1], Ksq[:, c * 512:(c + 1) * 512], start=True, stop=True), r=["Ksq", "onescb"], w=[("ps", c % 2)])
                    P.op("dve", lambda e, c=c: e.reduce_max(out=kmx[0:1, c:c + 1], in_=PS[0:1, c % 2, :], axis=AX.X), r=[("ps", c % 2)], w=["kmx"])
                P.op("dve", lambda e: e.reduce_max(out=kmx[0:1, 15:16], in_=kmx[0:1, 0:TK // 512], axis=AX.X), r=["kmx"], w=["kmx"])
                P.op("pe", lambda e: e.matmul(PS[:, 3, 300:301], onesc[0:1, 0:128], kmx[0:1, 15:16], start=True, stop=True), r=["kmx", "onesc"], w=[("ps", 3)])
                P.op("act", lambda e: e.copy(kb[:, 0:1], PS[:, 3, 300:301]), r=[("ps", 3)], w=["kb"])
                P.op("dve", lambda e: e.tensor_scalar(Mq[:, 0, :], pn, kb[:, 0:1], 1.0201, ALU.mult, ALU.mult), r=[("ps", 3), "kb"], w=[("Mq", hp)])
                P.op("act", lambda e: e.sqrt(Mq[:, 1, :], Mq[:, 0, :]), r=[("Mq", hp)], w=[("Mq", hp)])
                P.op("dve", lambda e: e.tensor_scalar(Mq[:, 1, :], Mq[:, 1, :], -1.0, None, ALU.mult), r=[("Mq", hp)], w=[("Mq", hp)])
                for t in range(NTO):
                    P.op("dve", lambda e, t=t: e.tensor_scalar(bias[:, t, :], bias[:, t, :], Mq[:, 1, t:t + 1], None, ALU.add), r=[("bias", hp), ("Mq", hp)], w=[("bias", hp)])

            def make_phases(h):
                hp = h % 2
                QTh = QT[:, hp, :]; KTh = KT[:, hp, :]; Vh = V[:, hp, :, :]
                bias = bias2[:, hp, :, :]; Mq = Mq2[:, hp, :, :]
                def S_phase(t, h=h, hp=hp, QTh=QTh, KTh=KTh):
                    par = t % 2
                    gj = NBP + t // 2
                    nblk = gj + 1
                    for n0 in range(0, nblk, 2):
                        nn = min(2, nblk - n0)
                        evc[0] += 1
                        bank = evc[0] % 2
                        for i in range(nn):
                            n = n0 + i
                            P.op("pe", lambda e, i=i, n=n, t=t, bank=bank: e.matmul(PS[:, bank, i * 256:(i + 1) * 256], QTh[:, t * 128:(t + 1) * 128], KTh[:, n * 256:(n + 1) * 256], start=True, stop=True),
                                 r=[("QT", hp, t // 4), ("KT", hp, n // 2)], w=[("ps", bank)])
                        for i in range(nn):
                            n = n0 + i
                            if n < gj:
                                P.op("act", lambda e, i=i, n=n, t=t, par=par, bank=bank: e.activation(out=Pb[:, par, n * 256:(n + 1) * 256], in_=PS[:, bank, i * 256:(i + 1) * 256], func=AF.Exp, bias=bias[:, t, n:n + 1], scale=1.0, accum_out=dparts[:, t, n:n + 1]),
                                     r=[("ps", bank), ("bias", hp)], w=[("Pb", par), ("dp", t)])
                            else:
                                oq = t % 2
                                P.op("dve", lambda e, i=i, t=t, oq=oq, bank=bank: e.tensor_tensor(otmp[:, oq, :], PS[:, bank, i * 256:(i + 1) * 256], cm[:, t % 2, :], ALU.add),
                                     r=[("ps", bank), "cm"], w=[("otmp", oq)])
                                P.op("act", lambda e, n=n, t=t, par=par, oq=oq: e.activation(out=Pb[:, par, n * 256:(n + 1) * 256], in_=otmp[:, oq, :], func=AF.Exp, bias=Mq[:, 1, t:t + 1], scale=1.0, accum_out=dparts[:, t, n:n + 1]),
                                     r=[("otmp", oq), ("Mq", hp)], w=[("Pb", par), ("dp", t)])
                    P.op("dve", lambda e, t=t, nblk=nblk: e.reduce_sum(out=sm[:, t, 2:3], in_=dparts[:, t, 0:nblk], axis=AX.X), r=[("dp", t)], w=[("sm", t)])

                def T_phase(t, h=h, hp=hp, Vh=Vh):
                    par = t % 2
                    gj = NBP + t // 2
                    nblk = gj + 1
                    nkt = 2 * nblk
                    po = PS[:, 3, 512 - 64:512]
                    for k0 in range(0, nkt, 8):
                        slot = ptc[0] % 2
                        ptc[0] += 1
                        tb = 2
                        PSx = PS[:, tb, :].bitcast(BF16)
                        ni = min(8, nkt - k0)
                        for i in range(ni):
                            kt = k0 + i
                            P.op("pe", lambda e, i=i, kt=kt, par=par, PSx=PSx: e.transpose(PSx[:, i * 128:(i + 1) * 128], Pb[:, par, kt * 128:(kt + 1) * 128], identb[:]), r=[("Pb", par), "identb"], w=[("ps", tb)])
                        if False:
                            P.op("act", lambda e, slot=slot, ni=ni, PSx=PSx: e.copy(PT[:, slot, 0:ni, :], PSx[:, 0:ni * 128].rearrange("p (i n) -> p i n", n=128)), r=[("ps", tb)], w=[("PT", slot)])
                        else:
                            P.op("dve", lambda e, slot=slot, ni=ni, PSx=PSx: e.tensor_copy(PT[:, slot, 0:ni, :], PSx[:, 0:ni * 128].rearrange("p (i n) -> p i n", n=128)), r=[("ps", tb)], w=[("PT", slot)])
                        for i in range(ni):
                            kt = k0 + i
                            P.op("pe", lambda e, i=i, kt=kt, slot=slot, nkt=nkt: e.matmul(po, PT[:, slot, i, :], Vh[:, kt, :], start=(kt == 0), stop=(kt == nkt - 1)),
                                 r=[("PT", slot), ("V", hp, kt // 8)], w=[("ps", 3)])
                    P.op("dve", lambda e, t=t: e.reciprocal(sm[:, t, 3:4], sm[:, t, 2:3]), r=[("sm", t)], w=[("sm", t)])
                    P.op("dve", lambda e, t=t, h=h: e.tensor_scalar(O[:, t, h * 64:(h + 1) * 64], po, sm[:, t, 3:4], None, ALU.mult), r=[("ps", 3), ("sm", t)], w=[("O", t)])
                return S_phase, T_phase

            prelude(0)
            for h in range(NH):
                S_phase, T_phase = make_phases(h)
                for t in range(NTO + 1):
                    if t == NTO - 1 and h + 1 < NH:
                        prelude(h + 1)
                    if os.environ.get('SKIPATT'): break
                    if t < NTO and not os.environ.get('SKIPS'):
                        S_phase(t)
                    if t >= 1 and not os.environ.get('SKIPT'):
                        T_phase(t - 1)
            P.barrier(); P.emit()
        s1.close()
        with ExitStack() as so:
            if X is None:
                if xstack is not None:
                    X = sbt(nc, xstack, "Xres", [128, NTO, 1024], F32, side="right")
                else:
                    X = sbt(nc, so, "a_X", [128, NTO, 1024], F32)
            Wo = sbt(nc, so, "a_Wo", [128, 8, 1024], BF16)
            OT = sbt(nc, so, "a_OT", [128, 8, 128], BF16)
            xtmp2 = sbt(nc, so, "a_xtmp2", [128, 2, 1024], F32)
            for c in range(8):
                P.op("pool", lambda e, c=c: e.dma_start(out=Wo[:, c, :], in_=wo[c * 128:(c + 1) * 128, :]), w=[("Wo", c)], dma=True, lane="wo%d" % (c % 4))
            for t in range(NTO):
                q = t % 2
                P.op("sp", lambda e, t=t, q=q: e.dma_start(out=xtmp2[:, q, :], in_=xkv[own0 + t * 128:own0 + (t + 1) * 128, :]), w=[("xtmp2", q)], dma=True, lane="ax%d" % q)
                for c in range(8):
                    P.op("pe", lambda e, t=t, c=c: e.transpose(PSb[:, c * 128:(c + 1) * 128], O[:, t, c * 128:(c + 1) * 128], identb[:]), r=[("O", t), "identb"], w=[("ps", 2)])
                P.op("act", lambda e: e.copy(OT[:], PSb[:].rearrange("p (c n) -> p c n", n=128)), r=[("ps", 2)], w=["OT"])
                for hf in range(2):
                    for c in range(8):
                        P.op("pe", lambda e, hf=hf, c=c: e.matmul(PS[:, hf, :], OT[:, c, :], Wo[:, c, hf * 512:(hf + 1) * 512], start=(c == 0), stop=(c == 7)),
                             r=["OT", ("Wo", c)], w=[("ps", hf)])
                P.op("dve", lambda e, t=t, q=q: e.scalar_tensor_tensor(out=X[:, t, :], in0=xtmp2[:, q, :], scalar=ALPHA, in1=PS[:, 0:2, :].rearrange("p b n -> p (b n)"), op0=ALU.mult, op1=ALU.add),
                     r=[("xtmp2", q), ("ps", 0), ("ps", 1)], w=[("X", t)])
            P.barrier(); P.emit()
            if lng is not None:
                with ExitStack() as sc:
                    layer_norm_out(P, nc, sc, X, lng, lnb, out_dram, "lna")
                    P.barrier(); P.emit()
    return X


def rope_consts():
    import numpy as np
    rc = np.zeros((128, 8), np.float32)
    for p in range(64):
        if p < 16:
            rc[p, 0] = 500000.0 ** (-(2 * (p % 8)) / 16.0)
            rc[p, 1] = -1.0 if p < 8 else 1.0
            rc[p, 2] = 1.0
            rc[p, 3] = 0.0
        else:
            rc[p, 0] = 0.0; rc[p, 1] = 0.0; rc[p, 2] = 0.0; rc[p, 3] = 1.0
    rc[:, 4] = -math.pi
    return rc


def attn_masks(NTK, NTO, has_prev):
    import numpy as np
    NB = NTK // 2; NBP = (NTK - NTO) // 2
    pm = np.full((NTO, NB), -1e30, np.float32)
    for t in range(NTO):
        gj = NBP + t // 2
        for n in range(NB):
            if n < gj and (has_prev or n >= NBP):
                pm[t, n] = 0.0
    pm = np.broadcast_to(pm.reshape(1, -1), (128, NTO * NB)).copy()
    cm = np.zeros((128, 2, 256), np.float32)
    for par in range(2):
        qpos = par * 128 + np.arange(128)[:, None]
        kpos = np.arange(256)[None, :]
        cm[:, par, :] = np.where(kpos <= qpos, 0.0, -BIG)
    return pm, cm.reshape(128, 512)


def build_attn_prog(NTK=32, NTO=16, NH=16, ln=True):
    nc = bass.Bass("TRN2", target_bir_lowering=False)
    dt = lambda name, shape, kind="ExternalInput", d=F32: nc.dram_tensor(name, shape, d, kind=kind).ap()
    xkv = dt("xkv", [NTK * 128, 1024]); pos = dt("pos", [1, NTK * 128], d=I32)
    identd = dt("ident", [128, 128])
    wqkv = dt("wqkv", [1024, 3072]); wo = dt("wo", [1024, 1024])
    ropec = dt("ropec", [128, 8]); pastmask = dt("pastmask", [128, NTO * (NTK // 2)]); causal = dt("causal", [128, 512])
    lng = dt("lng", [1, 1024]); lnb = dt("lnb", [1, 1024])
    y = dt("y", [NTO * 128, 1024], "ExternalOutput")
    with ExitStack() as st:
        P = Prog(nc, st)
        ident = sbt(nc, st, "identsb", [128, 128], F32)
        PS = st.enter_context(nc.psum_tensor("PS", [128, 8, 512], F32))
        P.op("sp", lambda e: e.dma_start(out=ident[:], in_=identd), w=["ident"], dma=True)
        attn_stage(P, nc, None, PS, ident, xkv, pos, wqkv, wo, ropec, pastmask, causal, lng if ln else None, lnb, y, NTK=NTK, NTO=NTO, NH=NH)
        if not ln:
            for tt in range(NTO):
                P.op("sp", lambda e, tt=tt: e.dma_start(out=y[tt * 128:(tt + 1) * 128, :], in_=X[:, tt, :]), r=[("X", tt)], w=[("out", tt)], dma=True, lane=P.rr_lane("out", 4))
            P.barrier(); P.emit()
        print("total ops", P.n_total)
    return nc
from contextlib import ExitStack

LN_EPS = 1e-5
KSCALE = 128 ** -0.5


def mlstm_stage(P, nc, X, PS, ident, xkv, w_in, b_gates, norm_g, w_out, consts, flag, lng, lnb, out_dram, NTK=32, NTO=16, x_in_sbuf=False, xkv_keys=(), prev_src=None):
    NTP = NTK - NTO
    PSb = PS[:, 2, :].bitcast(BF16)
    with ExitStack() as st:
        W = sbt(nc, st, "m_W", [128, 8, 3080], BF16)
        Wo = sbt(nc, st, "m_Wo", [128, 8, 1024], BF16)
        CN = sbt(nc, st, "m_CN", [128, 1280], F32)
        Tri = CN[:, 0:128]; I4 = CN[:, 128:640].rearrange("p (h t) -> p h t", t=128); Tri4 = CN[:, 640:1152].rearrange("p (h t) -> p h t", t=128); ones = CN[:, 1152:1280]
        Cst = sbt(nc, st, "m_C", [128, 4, 256], F32)
        nst = sbt(nc, st, "m_n", [128, 4], F32)
        car = sbt(nc, st, "m_car", [128, 2, 4], F32)
        bg = sbt(nc, st, "m_bg", [128, 8], F32)
        ng = sbt(nc, st, "m_ng", [128, 1024], F32)
        flg = sbt(nc, st, "m_flag", [128, 1], F32)
        identb = sbt(nc, st, "m_identb", [128, 128], BF16)
        xtmp = sbt(nc, st, "m_xtmp", [128, 1024], F32)
        xTc = sbt(nc, st, "m_xTc", [128, 8, 128], BF16)
        ktm = sbt(nc, st, "m_ktm", [128, 4, 128], F32)
        v = sbt(nc, st, "m_v", [128, 4, 256], F32)
        og = sbt(nc, st, "m_og", [128, 1024], F32)
        qT = sbt(nc, st, "m_qT", [128, 4, 128], F32)
        kT = sbt(nc, st, "m_kT", [128, 4, 128], F32)
        g = sbt(nc, st, "m_g", [128, 8, 4], F32)
        abc = sbt(nc, st, "m_abc", [128, 4, 128], F32)
        arep = sbt(nc, st, "m_arep", [128, 4, 128], F32)
        Mrep = sbt(nc, st, "m_Mrep", [128, 4, 128], F32)
        WT = sbt(nc, st, "m_WT", [128, 4, 128], F32)
        AT = sbt(nc, st, "m_AT", [128, 4, 128], F32)
        qp = sbt(nc, st, "m_qp", [128, 4, 128], F32)
        hh = sbt(nc, st, "m_h", [128, 4, 256], F32)
        hsq = sbt(nc, st, "m_hsq", [128, 4, 256], F32)
        hg = sbt(nc, st, "m_hg", [128, 1024], BF16)
        hgT = sbt(nc, st, "m_hgT", [128, 8, 128], BF16)
        ks = sbt(nc, st, "m_ks", [128, 4, 128], F32)
        s8 = sbt(nc, st, "m_s8", [128, 8, 4], F32)
        for k in range(8):
            P.op("pool", lambda e, k=k: e.dma_start(out=W[:, k, :], in_=w_in[k * 128:(k + 1) * 128, :]), w=[("W", k)], dma=True, lane="mw%d" % (k % 4))
        for k in range(8):
            P.op("pool", lambda e, k=k: e.dma_start(out=Wo[:, k, :], in_=w_out[k * 128:(k + 1) * 128, :]), w=[("Wo", k)], dma=True, lane="mw%d" % (k % 4))
        Wk = [("W", k) for k in range(8)]
        Wok = [("Wo", k) for k in range(8)]
        P.op("sp", lambda e: e.dma_start(out=CN[:], in_=consts), w=["CN"], dma=True)
        P.op("sp", lambda e: e.dma_start(out=bg[:], in_=b_gates.partition_broadcast(128)), w=["bg"], dma=True)
        P.op("sp", lambda e: e.dma_start(out=ng[:], in_=norm_g.partition_broadcast(128)), w=["ng"], dma=True)
        P.op("sp", lambda e: e.dma_start(out=flg[:], in_=flag), w=["flag"], dma=True)
        P.op("dve", lambda e: e.tensor_copy(identb[:], ident[:]), r=["ident"], w=["identb"])
        P.op("dve", lambda e: e.memset(Cst[:], 0.0), w=["C"])
        P.op("dve", lambda e: e.memset(nst[:], 0.0), w=["n"])
        P.op("dve", lambda e: e.memset(car[:], 0.0), w=["car"])
        ptr = PS[:, 0:2, :].rearrange("p b (k n) -> p (b k) n", n=128)
        for c in range(NTK):
            own = c >= NTP
            co = c - NTP
            if own:
                xsrc = X[:, co, :]
                xkey = ("X", co)
                if not x_in_sbuf:
                    P.op("sp", lambda e, c=c, co=co: e.dma_start(out=X[:, co, :], in_=xkv[c * 128:(c + 1) * 128, :]), w=[xkey], dma=True, lane=P.rr_lane("mx", 2))
            else:
                xsrc = xtmp[:]
                xkey = "xtmp"
                src_ap = prev_src(c) if prev_src is not None else xkv[c * 128:(c + 1) * 128, :]
                P.op("sp", lambda e, src_ap=src_ap: e.dma_start(out=xtmp[:], in_=src_ap), r=list(xkv_keys), w=[xkey], dma=True, lane=P.rr_lane("mx", 2))
            for k in range(8):
                P.op("pe", lambda e, k=k, xsrc=xsrc: e.transpose(ptr[:, k, :], xsrc[:, k * 128:(k + 1) * 128], ident[:]), r=[xkey, "ident"], w=[("ps", k // 4)])
            P.op("act", lambda e: e.copy(xTc[:], ptr), r=[("ps", 0), ("ps", 1)], w=["xTc"])
            def tm_proj(bank, c0, n):
                for k in range(8):
                    P.op("pe", lambda e, k=k, bank=bank, c0=c0, n=n: e.matmul(PS[:, bank, 0:n], xTc[:, k, :], W[:, k, c0:c0 + n], start=(k == 0), stop=(k == 7)),
                         r=["xTc", ("W", k)], w=[("ps", bank)])
            tm_proj(0, 512, 512)
            P.op("act", lambda e: e.activation(out=ktm[:].rearrange("p h d -> p (h d)"), in_=PS[:, 0, :], func=AF.Copy, scale=KSCALE), r=[("ps", 0)], w=["ktm"])
            tm_proj(1, 1024, 512)
            P.op("act", lambda e: e.copy(v[:, 0:2, :].rearrange("p h d -> p (h d)"), PS[:, 1, :]), r=[("ps", 1)], w=["v"])
            tm_proj(0, 1536, 512)
            P.op("act", lambda e: e.copy(v[:, 2:4, :].rearrange("p h d -> p (h d)"), PS[:, 0, :]), r=[("ps", 0)], w=["v"])
            if own:
                tm_proj(1, 2048, 512)
                P.op("act", lambda e: e.activation(out=og[:, 0:512], in_=PS[:, 1, :], func=AF.Sigmoid), r=[("ps", 1)], w=["og"])
                tm_proj(0, 2560, 512)
                P.op("act", lambda e: e.activation(out=og[:, 512:1024], in_=PS[:, 0, :], func=AF.Sigmoid), r=[("ps", 0)], w=["og"])
            tm_proj(3, 3072, 8)
            P.op("dve", lambda e: e.tensor_tensor(g[:, 0:2, :].rearrange("p a h -> p (a h)"), PS[:, 3, 0:8], bg[:], ALU.add), r=[("ps", 3), "bg"], w=["g01"])
            P.op("act", lambda e: e.activation(out=g[:, 5, :], in_=g[:, 1, :], func=AF.Exp, scale=-1.0), r=["g01"], w=["g5"])
            P.op("act", lambda e: e.activation(out=g[:, 6, :], in_=g[:, 5, :], func=AF.Ln, bias=ones[:, 0:1], scale=1.0), r=["g5", "CN"], w=["g6"])
            P.op("dve", lambda e: e.tensor_scalar(g[:, 1, :], g[:, 6, :], -1.0, None, ALU.mult), r=["g6", "g01"], w=["g01"])
            P.op("pe", lambda e: e.matmul(PS[:, 3, 8:12], Tri, g[:, 1, :], start=True, stop=True), r=["CN", "g01"], w=[("ps", 3)])
            P.op("dve", lambda e: e.tensor_tensor(g[:, 2, :], PS[:, 3, 8:12], car[:, 0, :], ALU.add), r=[("ps", 3), "car"], w=["g2"])
            P.op("dve", lambda e: e.tensor_tensor(g[:, 3, :], g[:, 0, :], g[:, 2, :], ALU.subtract), r=["g01", "g2"], w=["g3"])
            P.op("pe", lambda e: e.matmul(PS[:, 3, 12:16], ones, g[:, 1, :], start=True, stop=True), r=["CN", "g01"], w=[("ps", 3)])
            P.op("dve", lambda e: e.tensor_tensor(car[:, 0, :], PS[:, 3, 12:16], car[:, 0, :], ALU.add), r=[("ps", 3), "car", "g2"], w=["car"])
            for hd in range(4):
                P.op("dve", lambda e, hd=hd: e.tensor_scalar(abc[:, hd, :], ones, g[:, 3, hd:hd + 1], None, ALU.mult), r=["CN", "g3"], w=["abc"])
            parep = PS[:, 3, :].rearrange("p (h t) -> p h t", t=128)
            for hd in range(4):
                P.op("pe", lambda e, hd=hd: e.matmul(parep[:, hd, :], abc[:, hd, :], I4[:, 0, :], start=True, stop=True), r=["abc", "CN"], w=[("ps", 3)])
            P.op("act", lambda e: e.copy(arep[:], parep), r=[("ps", 3)], w=["arep"])
            P.op("dve", lambda e: e.tensor_copy(s8[:, 0, :], car[:, 1, :]), r=["car"], w=["s80"])
            for hd in range(4):
                P.op("dve", lambda e, hd=hd: e.tensor_tensor_scan(out=Mrep[:, hd, :], data0=arep[:, hd, :], data1=arep[:, hd, :], initial=s8[:, 0, hd:hd + 1], op0=ALU.max, op1=ALU.max),
                     r=["arep", "s80"], w=["Mrep"])
            P.op("dve", lambda e: e.tensor_copy(car[:, 1, :], Mrep[:, :, 127]), r=["Mrep", "s80"], w=["car"])
            P.op("dve", lambda e: e.tensor_tensor(s8[:, 1, :], g[:, 3, :], car[:, 1, :], ALU.subtract), r=["g3", "car"], w=["s81"])
            P.op("dve", lambda e: e.tensor_tensor(s8[:, 2, :], s8[:, 0, :], car[:, 1, :], ALU.subtract), r=["s80", "car"], w=["s82"])
            P.op("act", lambda e: e.activation(out=s8[:, 3:5, :], in_=s8[:, 1:3, :], func=AF.Exp), r=["s81", "s82"], w=["s834"])
            if own:
                pq = PS[:, 2, :].rearrange("p (h t) -> p h t", t=128)
                for (c0, dst, scl, nm) in ((0, qT, 1.0, "qT"), (512, kT, KSCALE, "kT")):
                    for hd in range(4):
                        for k in range(8):
                            P.op("pe", lambda e, hd=hd, k=k, c0=c0: e.matmul(pq[:, hd, :], W[:, k, c0 + hd * 128:c0 + (hd + 1) * 128], xTc[:, k, :], start=(k == 0), stop=(k == 7)),
                                 r=["xTc", ("W", k)], w=[("ps", 2)])
                    P.op("act", lambda e, dst=dst, scl=scl: e.activation(out=dst[:], in_=pq, func=AF.Copy, scale=scl), r=[("ps", 2)], w=[nm])
                P.op("dve", lambda e: e.tensor_tensor(WT[:], Mrep[:], I4, ALU.mult), r=["Mrep", "CN"], w=["WT"])
                P.op("dve", lambda e: e.tensor_reduce(out=g[:, 4, :], in_=WT[:], axis=AX.X, op=ALU.add), r=["WT"], w=["g4"])
                P.op("dve", lambda e: e.tensor_tensor(g[:, 7, :], g[:, 2, :], g[:, 4, :], ALU.add), r=["g2", "g4"], w=["g7"])
                P.op("act", lambda e: e.activation(out=s8[:, 5, :], in_=g[:, 7, :], func=AF.Exp, scale=-1.0), r=["g7"], w=["s85"])
                for hd in range(4):
                    P.op("dve", lambda e, hd=hd: e.tensor_scalar(WT[:, hd, :], Mrep[:, hd, :], g[:, 3, hd:hd + 1], 0.0, ALU.subtract, ALU.max), r=["Mrep", "g3", "g4"], w=["WT"])
                P.op("act", lambda e: e.activation(out=WT[:], in_=WT[:], func=AF.Exp, scale=-1.0), r=["WT"], w=["WT"])
                P.op("dve", lambda e: e.tensor_tensor(WT[:], WT[:], Tri4, ALU.mult), r=["WT", "CN"], w=["WT"])
                for hd in range(4):
                    P.op("pe", lambda e, hd=hd: e.matmul(pq[:, hd, :], kT[:, hd, :], qT[:, hd, :], start=True, stop=True), r=["kT", "qT"], w=[("ps", 2)])
                P.op("dve", lambda e: e.tensor_tensor(AT[:], WT[:], pq, ALU.mult), r=["WT", ("ps", 2)], w=["AT"])
                for hd in range(4):
                    P.op("act", lambda e, hd=hd: e.activation(out=qp[:, hd, :], in_=Mrep[:, hd, :], func=AF.Exp, bias=s8[:, 0, hd:hd + 1], scale=-1.0), r=["Mrep", "s80"], w=["qp"])
                P.op("dve", lambda e: e.tensor_tensor(qp[:], qp[:], qT[:], ALU.mult), r=["qp", "qT"], w=["qp"])
                for hd in range(4):
                    po = PS[:, hd // 2, (hd % 2) * 256:(hd % 2 + 1) * 256]
                    P.op("pe", lambda e, hd=hd, po=po: e.matmul(po, qp[:, hd, :], Cst[:, hd, :], start=True, stop=False), r=["qp", "C"], w=[("ps", hd // 2)])
                    P.op("pe", lambda e, hd=hd, po=po: e.matmul(po, AT[:, hd, :], v[:, hd, :], start=False, stop=True), r=["AT", "v"], w=[("ps", hd // 2)])
                for hd in range(4):
                    pd = PS[:, 3, 16 + hd:17 + hd]
                    P.op("pe", lambda e, hd=hd, pd=pd: e.matmul(pd, qp[:, hd, :], nst[:, hd:hd + 1], start=True, stop=False), r=["qp", "n"], w=[("ps", 3)])
                    P.op("pe", lambda e, hd=hd, pd=pd: e.matmul(pd, AT[:, hd, :], ones[:, 0:1], start=False, stop=True), r=["AT", "CN"], w=[("ps", 3)])
                P.op("dve", lambda e: e.tensor_scalar(s8[:, 6, :], PS[:, 3, 16:20], -1.0, None, ALU.mult), r=[("ps", 3)], w=["s86"])
                P.op("dve", lambda e: e.tensor_tensor(s8[:, 6, :], s8[:, 6, :], PS[:, 3, 16:20], ALU.max), r=[("ps", 3), "s86"], w=["s86"])
                P.op("dve", lambda e: e.tensor_tensor(s8[:, 6, :], s8[:, 6, :], s8[:, 5, :], ALU.max), r=["s86", "s85"], w=["s86"])
                P.op("dve", lambda e: e.reciprocal(s8[:, 7, :], s8[:, 6, :]), r=["s86"], w=["s87"])
                for hd in range(4):
                    po = PS[:, hd // 2, (hd % 2) * 256:(hd % 2 + 1) * 256]
                    P.op("dve", lambda e, hd=hd, po=po: e.tensor_scalar(hh[:, hd, :], po, s8[:, 7, hd:hd + 1], None, ALU.mult), r=[("ps", hd // 2), "s87"], w=["hh"])
                P.op("dve", lambda e: e.tensor_reduce(out=g[:, 5, :], in_=hh[:], axis=AX.X, op=ALU.add), r=["hh", "g5"], w=["g5"])
                P.op("act", lambda e: e.activation(out=hsq[:], in_=hh[:], func=AF.Square), r=["hh"], w=["hsq"])
                P.op("dve", lambda e: e.tensor_reduce(out=g[:, 6, :], in_=hsq[:], axis=AX.X, op=ALU.add), r=["hsq", "g6"], w=["g6"])
                P.op("dve", lambda e: e.tensor_scalar(g[:, 5:7, :], g[:, 5:7, :], 1.0 / 256, None, ALU.mult), r=["g5", "g6"], w=["g5", "g6"])
                P.op("dve", lambda e: e.tensor_tensor(s8[:, 1, :], g[:, 5, :], g[:, 5, :], ALU.mult), r=["g5", "s81"], w=["s81"])
                P.op("dve", lambda e: e.tensor_tensor(s8[:, 1, :], g[:, 6, :], s8[:, 1, :], ALU.subtract), r=["g6", "s81"], w=["s81"])
                P.op("dve", lambda e: e.tensor_scalar(s8[:, 1, :], s8[:, 1, :], LN_EPS, None, ALU.add), r=["s81"], w=["s81"])
                P.op("act", lambda e: e.sqrt(s8[:, 2, :], s8[:, 1, :]), r=["s81", "s82"], w=["s82"])
                P.op("dve", lambda e: e.reciprocal(s8[:, 1, :], s8[:, 2, :]), r=["s82"], w=["s81"])
                for hd in range(4):
                    P.op("dve", lambda e, hd=hd: e.tensor_scalar(hh[:, hd, :], hh[:, hd, :], g[:, 5, hd:hd + 1], s8[:, 1, hd:hd + 1], ALU.subtract, ALU.mult), r=["hh", "g5", "s81"], w=["hh"])
                hflat = hh[:].rearrange("p h d -> p (h d)")
                P.op("dve", lambda e: e.tensor_tensor(hflat, hflat, ng[:], ALU.mult), r=["hh", "ng"], w=["hh"])
                P.op("dve", lambda e: e.tensor_tensor(hg[:], hflat, og[:], ALU.mult), r=["hh", "og"], w=["hg"])
                for k in range(8):
                    P.op("pe", lambda e, k=k: e.transpose(PSb[:, k * 128:(k + 1) * 128], hg[:, k * 128:(k + 1) * 128], identb[:]), r=["hg", "identb"], w=[("ps", 2)])
                P.op("act", lambda e: e.copy(hgT[:], PSb[:].rearrange("p (c n) -> p c n", n=128)), r=[("ps", 2)], w=["hgT"])
                for hf in range(2):
                    for k in range(8):
                        P.op("pe", lambda e, hf=hf, k=k: e.matmul(PS[:, hf, :], hgT[:, k, :], Wo[:, k, hf * 512:(hf + 1) * 512], start=(k == 0), stop=(k == 7)), r=["hgT", ("Wo", k)], w=[("ps", hf)])
                P.op("dve", lambda e, co=co: e.scalar_tensor_tensor(out=X[:, co, :], in0=X[:, co, :], scalar=ALPHA, in1=PS[:, 0:2, :].rearrange("p b n -> p (b n)"), op0=ALU.mult, op1=ALU.add),
                     r=[("X", co), ("ps", 0), ("ps", 1)], w=[("X", co)])
            for hd in range(4):
                P.op("dve", lambda e, hd=hd: e.tensor_scalar(ks[:, hd, :], ktm[:, hd, :], s8[:, 3, hd:hd + 1], None, ALU.mult), r=["ktm", "s834"], w=["ks"])
            for hd in range(4):
                po = PS[:, hd // 2, (hd % 2) * 256:(hd % 2 + 1) * 256]
                P.op("pe", lambda e, hd=hd, po=po: e.matmul(po, ks[:, hd, :], v[:, hd, :], start=True, stop=True), r=["ks", "v"], w=[("ps", hd // 2)])
            for hd in range(4):
                P.op("pe", lambda e, hd=hd: e.matmul(PS[:, 3, 24 + hd:25 + hd], ks[:, hd, :], ones[:, 0:1], start=True, stop=True), r=["ks", "CN"], w=[("ps", 3)])
            for hd in range(4):
                po = PS[:, hd // 2, (hd % 2) * 256:(hd % 2 + 1) * 256]
                P.op("dve", lambda e, hd=hd, po=po: e.scalar_tensor_tensor(out=Cst[:, hd, :], in0=Cst[:, hd, :], scalar=s8[:, 4, hd:hd + 1], in1=po, op0=ALU.mult, op1=ALU.add),
                     r=["C", "s834", ("ps", hd // 2)], w=["C"])
            P.op("dve", lambda e: e.tensor_tensor(nst[:], nst[:], s8[:, 4, :], ALU.mult), r=["n", "s834"], w=["n"])
            P.op("dve", lambda e: e.tensor_tensor(nst[:], nst[:], PS[:, 3, 24:28], ALU.add), r=["n", ("ps", 3)], w=["n"])
            if c == NTP - 1:
                P.op("dve", lambda e: e.tensor_scalar(Cst[:].rearrange("p h d -> p (h d)"), Cst[:].rearrange("p h d -> p (h d)"), flg[:, 0:1], None, ALU.mult), r=["C", "flag"], w=["C"])
                P.op("dve", lambda e: e.tensor_scalar(nst[:], nst[:], flg[:, 0:1], None, ALU.mult), r=["n", "flag"], w=["n"])
                P.op("dve", lambda e: e.tensor_scalar(car[:].rearrange("p a h -> p (a h)"), car[:].rearrange("p a h -> p (a h)"), flg[:, 0:1], None, ALU.mult), r=["car", "flag"], w=["car"])
        P.barrier(); P.emit()
    if lng is not None:
        with ExitStack() as sc:
            layer_norm_out(P, nc, sc, X, lng, lnb, out_dram, "lnm")
            P.barrier(); P.emit()


def mlstm_consts():
    import numpy as np
    cn = np.zeros((128, 1280), np.float32)
    tri = (np.arange(128)[:, None] <= np.arange(128)[None, :]).astype(np.float32)
    eye = np.eye(128, dtype=np.float32)
    cn[:, 0:128] = tri
    for h in range(4):
        cn[:, 128 + h * 128:128 + (h + 1) * 128] = eye
        cn[:, 640 + h * 128:640 + (h + 1) * 128] = tri
    cn[:, 1152:1280] = 1.0
    return cn


def build_mlstm_prog(NTK=32, NTO=16, ln=True):
    nc = bass.Bass("TRN2", target_bir_lowering=False)
    dt = lambda name, shape, kind="ExternalInput", d=F32: nc.dram_tensor(name, shape, d, kind=kind).ap()
    xkv = dt("xkv", [NTK * 128, 1024]); identd = dt("ident", [128, 128])
    w_in = dt("w_in", [1024, 3080]); b_gates = dt("b_gates", [1, 8]); norm_g = dt("norm_g", [1, 1024]); w_out = dt("w_out", [1024, 1024])
    consts = dt("consts", [128, 1280]); flag = dt("flag", [128, 1])
    lng = dt("lng", [1, 1024]); lnb = dt("lnb", [1, 1024])
    y = dt("y", [NTO * 128, 1024], "ExternalOutput")
    with ExitStack() as st:
        P = Prog(nc, st)
        X = sbt(nc, st, "X", [128, NTO, 1024], F32)
        ident = sbt(nc, st, "identsb", [128, 128], F32)
        PS = st.enter_context(nc.psum_tensor("PS", [128, 8, 512], F32))
        P.op("sp", lambda e: e.dma_start(out=ident[:], in_=identd), w=["ident"], dma=True)
        mlstm_stage(P, nc, X, PS, ident, xkv, w_in, b_gates, norm_g, w_out, consts, flag, lng if ln else None, lnb, y, NTK=NTK, NTO=NTO)
        print("total ops", P.n_total)
    return nc
from contextlib import ExitStack


def build_fused_prog(E=32, NH=16, NTO=16, groups=None):
    NTK = 2 * NTO; TO = NTO * 128; TK = NTK * 128
    if groups is None:
        groups = [[0, 1], [2, 3], [4, 5], [6, 7]]
    nc = bass.Bass("TRN2", target_bir_lowering=False)
    dt = lambda name, shape, kind="ExternalInput", d=F32: nc.dram_tensor(name, shape, d, kind=kind).ap()
    xkv = dt("xkv", [TK, 1024]); pos = dt("pos", [1, TK], d=I32); identd = dt("ident", [128, 128])
    wqkv = dt("wqkv", [1024, 3072]); wo = dt("wo", [1024, 1024])
    ropec = dt("ropec", [128, 8]); pastmask = dt("pastmask", [128, NTO * (NTK // 2)]); causal = dt("causal", [128, 512])
    w_in = dt("w_in", [1024, 3080]); b_gates = dt("b_gates", [1, 8]); norm_g = dt("norm_g", [1, 1024]); w_out = dt("w_out", [1024, 1024])
    consts = dt("consts", [128, 1280]); flag = dt("flag", [128, 1])
    lnmg = [dt("lnmg%d" % L, [1, 1024]) for L in range(2)]; lnmb = [dt("lnmb%d" % L, [1, 1024]) for L in range(2)]
    lnfg = [dt("lnfg%d" % L, [1, 1024]) for L in range(2)]; lnfb = [dt("lnfb%d" % L, [1, 1024]) for L in range(2)]
    rw = [dt("rw%d" % L, [1024, E]) for L in range(2)]; rb = [dt("rb%d" % L, [1, E]) for L in range(2)]
    wgu = [dt("wgu%d" % L, [E, 1024, 2048]) for L in range(2)]; bgu = [dt("bgu%d" % L, [E, 2048]) for L in range(2)]
    wd = [dt("wd%d" % L, [E, 1024, 1024]) for L in range(2)]; bd = [dt("bd%d" % L, [E, 1024]) for L in range(2)]
    y = dt("y", [TO, 1024], "ExternalOutput")
    xch_in = nc.dram_tensor("xch_in", [TO // 512, 512, 1024], F32)
    xch_out = nc.dram_tensor("xch_out", [TO // 512, 1024, 1024], F32)
    with ExitStack() as st:
        P = Prog(nc, st)
        ident = sbt(nc, st, "identsb", [128, 128], F32)
        PS = st.enter_context(nc.psum_tensor("PS", [128, 8, 512], F32))
        P.op("sp", lambda e: e.dma_start(out=ident[:], in_=identd), w=["ident"], dma=True)
        X = attn_stage(P, nc, None, PS, ident, xkv, pos, wqkv, wo, ropec, pastmask, causal, lnmg[0], lnmb[0], None, NTK=NTK, NTO=NTO, NH=NH, xstack=st)
        moe_stage(P, nc, X, PS, ident, rw[0], rb[0], wgu[0], bgu[0], wd[0], bd[0], lnfg[0], lnfb[0], None, E=E)
        NCH = TO // 512
        for tt in range(NTO):
            j, r = divmod(tt, 4)
            P.op("sp", lambda e, tt=tt, j=j, r=r: e.dma_start(out=xch_in.ap()[j, r * 128:(r + 1) * 128, :], in_=X[:, tt, :]), r=[("X", tt)], w=[("xin", tt)], dma=True, lane=P.rr_lane("xo", 4))
        for j in range(NCH):
            P.op("pool", lambda e, j=j: e.collective_compute("AllGather", ALU.bypass, replica_groups=groups, ins=[xch_in.ap()[j].opt()], outs=[xch_out.ap()[j].opt()]),
                 r=[("xin", tt) for tt in range(4 * j, 4 * j + 4)], w=[("xout", j)], dma=True, lane="cc", inc=1)
        P.barrier(); P.emit()
        mlstm_stage(P, nc, X, PS, ident, None, w_in, b_gates, norm_g, w_out, consts, flag, lnmg[1], lnmb[1], None, NTK=NTK, NTO=NTO, x_in_sbuf=True, xkv_keys=[("xout", j) for j in range(TO // 512)], prev_src=lambda c: xch_out.ap()[c // 4, (c % 4) * 128:(c % 4 + 1) * 128, :])
        moe_stage(P, nc, X, PS, ident, rw[1], rb[1], wgu[1], bgu[1], wd[1], bd[1], lnfg[1], lnfb[1], y, E=E)
        print("total ops", P.n_total)
    return nc

_PROGS = {}


def kernel(x, positions, attn_w_qkv, attn_w_o, mlstm_w_in, mlstm_b_gates, mlstm_norm_g, mlstm_w_out,
           ln_mix_g, ln_mix_b, ln_ffn_g, ln_ffn_b, router_w, router_b, w_gate_up, b_gate_up, w_down, b_down):
    f32 = lambda a: np.ascontiguousarray(np.asarray(a, dtype=np.float32))
    x = f32(x)
    positions = np.ascontiguousarray(np.asarray(positions, dtype=np.int32))
    ident = np.eye(128, dtype=np.float32)
    H = 2048
    row = lambda a: f32(a).reshape(1, -1)
    if "fused" not in _PROGS:
        _PROGS["fused"] = build_fused_prog()
    nc = _PROGS["fused"]
    rc = rope_consts()
    cn = mlstm_consts()
    shared = dict(ident=ident, wqkv=f32(attn_w_qkv[0]), wo=f32(attn_w_o[0]), ropec=rc,
                  w_in=f32(mlstm_w_in[0]), b_gates=row(mlstm_b_gates[0]), norm_g=row(mlstm_norm_g[0]), w_out=f32(mlstm_w_out[0]), consts=cn)
    for L in range(2):
        shared.update({"lnmg%d" % L: row(ln_mix_g[L]), "lnmb%d" % L: row(ln_mix_b[L]), "lnfg%d" % L: row(ln_ffn_g[L]), "lnfb%d" % L: row(ln_ffn_b[L]),
                       "rw%d" % L: f32(router_w[L]), "rb%d" % L: row(router_b[L]), "wgu%d" % L: f32(w_gate_up[L]), "bgu%d" % L: f32(b_gate_up[L]),
                       "wd%d" % L: f32(w_down[L]), "bd%d" % L: f32(b_down[L])})
    maps = []
    for c in range(8):
        b, h = divmod(c, 2)
        if h == 1:
            xkv = x[b]; pos = positions[b]
        else:
            xkv = np.concatenate([x[b, :H], x[b, :H]], 0); pos = np.concatenate([positions[b, :H], positions[b, :H]], 0)
        pm, cm = attn_masks(32, 16, h == 1)
        d = dict(shared)
        d.update(xkv=np.ascontiguousarray(xkv), pos=np.ascontiguousarray(pos).reshape(1, -1), pastmask=pm, causal=cm,
                 flag=np.full((128, 1), float(h), np.float32))
        maps.append(d)
    res = run_bass_kernel_spmd(nc, maps, core_ids=list(range(8)))
    ys = [r["y"] for r in res.results]
    out = np.stack([np.concatenate([ys[2 * b], ys[2 * b + 1]], 0) for b in range(4)], 0)
    return out.astype(np.float32)
```

```python
import os, sys, math
from contextlib import ExitStack
import numpy as np
from concourse.bass_utils import run_bass_kernel_spmd

import numpy as np
import concourse.bass as bass
import concourse.mybir as mybir

F32 = mybir.dt.float32
BF16 = mybir.dt.bfloat16
I32 = mybir.dt.int32
AF = mybir.ActivationFunctionType
ALU = mybir.AluOpType
AX = mybir.AxisListType

ENGS = ("pe", "act", "dve", "pool", "sp")


class Prog:
    def __init__(self, nc, stack):
        self.nc = nc
        self.stack = stack
        self.eng = {"pe": nc.tensor, "act": nc.scalar, "dve": nc.vector, "pool": nc.gpsimd, "sp": nc.sync}
        self.sem = {}
        for e in ENGS:
            self.sem[e] = stack.enter_context(nc.semaphore("s_" + e))
        self.cnt = {k: 0 for k in self.sem}
        self.known = {e: {k: 0 for k in self.sem} for e in ENGS}
        self.lane_rr = {}
        self.last_w = {}
        self.readers = {}
        self.ops = []
        self.n_total = 0

    def lane(self, name):
        key = "L:" + name
        if key not in self.sem:
            self.sem[key] = self.stack.enter_context(self.nc.semaphore("sl_" + name))
            self.cnt[key] = 0
            for e in ENGS:
                self.known[e][key] = 0
        return key

    def rr_lane(self, prefix, n):
        i = self.lane_rr.get(prefix, 0)
        self.lane_rr[prefix] = i + 1
        return "%s%d" % (prefix, i % n)

    def op(self, e, fn, r=(), w=(), dma=False, lane=None, inc=None):
        r2, w2 = [], list(w)
        for k in r:
            if isinstance(k, tuple) and k[0] == "ps":
                w2.append(k)
            else:
                r2.append(k)
        w = [(("ps", k[1] % 4) if (isinstance(k, tuple) and k[0] == "ps") else k) for k in w2]
        w = list(dict.fromkeys(w))
        r = r2
        deps = {}
        def add(tok):
            if tok is None:
                return
            s, v = tok
            if deps.get(s, 0) < v:
                deps[s] = v
        for k in r:
            add(self.last_w.get(k))
        for k in w:
            add(self.last_w.get(k))
            for t in self.readers.get(k, ()):
                add(t)
        if dma:
            semname = self.lane(lane if lane is not None else self.rr_lane("misc", 4))
            if self.cnt[semname] > 0:
                add((semname, self.cnt[semname]))
        else:
            semname = e
        step = inc if inc is not None else (16 if dma else 1)
        self.cnt[semname] += step
        tok = (semname, self.cnt[semname])
        for k in r:
            self.readers.setdefault(k, []).append(tok)
        for k in w:
            self.last_w[k] = tok
            self.readers[k] = []
        waits = []
        kn = self.known[e]
        for s, v in deps.items():
            if s == e:
                if e == "pe":
                    continue
                if self.cnt[e] - 1 - v >= 3:
                    continue
            if kn[s] >= v:
                continue
            kn[s] = v
            waits.append((s, v))
        self.ops.append((e, fn, waits, semname, step))
        self.n_total += 1
        return tok

    def barrier(self):
        tot = dict(self.cnt)
        for e in ENGS:
            waits = []
            for s, v in tot.items():
                if v > self.known[e][s] and s != e:
                    self.known[e][s] = v
                    waits.append((s, v))
            if waits:
                self.ops.append((e, None, waits, None, 0))

    def emit(self):
        nc = self.nc
        ops = self.ops
        if not any(o[1] is not None for o in ops):
            return
        self.ops = []
        sem = self.sem
        with nc.Block() as block:
            def make(ename):
                def body(engine):
                    for (e, fn, waits, semname, inc) in ops:
                        if e != ename:
                            continue
                        for s, v in waits:
                            engine.wait_ge(sem[s], v)
                        if fn is not None:
                            ins = fn(engine)
                            ins.then_inc(sem[semname], inc)
                return body
            block.tensor(make("pe"))
            block.scalar(make("act"))
            block.vector(make("dve"))
            block.gpsimd(make("pool"))
            block.sync(make("sp"))

from contextlib import ExitStack

ALPHA = 4 ** 0.25
LN_EPS = 1e-5
NTT = 16

_SBT_N = [0]


def sbt(nc, st, name, shape, dt, side=None):
    _SBT_N[0] += 1
    nm = "%s_%d" % (name, _SBT_N[0])
    if side is None:
        return st.enter_context(nc.sbuf_tensor(nm, shape, dt))
    return st.enter_context(nc.sbuf_tensor(nm, shape, dt, side=side))


def layer_norm_out(P, nc, st, X, lng, lnb, out_dram, tag, xdst=None):
    G = sbt(nc, st, tag + "G", [128, 1024], F32)
    B = sbt(nc, st, tag + "B", [128, 1024], F32)
    junk = sbt(nc, st, tag + "junk", [128, 1024], F32)
    stat = sbt(nc, st, tag + "stat", [128, NTT, 8], F32)
    P.op("sp", lambda e: e.dma_start(out=G[:], in_=lng.partition_broadcast(128)), w=[tag + "G"], dma=True)
    P.op("sp", lambda e: e.dma_start(out=B[:], in_=lnb.partition_broadcast(128)), w=[tag + "B"], dma=True)
    for tt in range(NTT):
        xk = ("X", tt)
        sk = (tag + "stat", tt)
        P.op("act", lambda e, tt=tt: e.activation(out=junk[:], in_=X[:, tt, :], func=AF.Identity, accum_out=stat[:, tt, 0:1]), r=[xk], w=[tag + "junk", sk])
        P.op("act", lambda e, tt=tt: e.activation(out=junk[:], in_=X[:, tt, :], func=AF.Square, accum_out=stat[:, tt, 1:2]), r=[xk], w=[tag + "junk", sk])
        P.op("dve", lambda e, tt=tt: e.tensor_scalar(stat[:, tt, 2:4], stat[:, tt, 0:2], 1.0 / 1024, None, ALU.mult), r=[sk], w=[sk])
        P.op("dve", lambda e, tt=tt: e.tensor_tensor(stat[:, tt, 4:5], stat[:, tt, 2:3], stat[:, tt, 2:3], ALU.mult), r=[sk], w=[sk])
        P.op("dve", lambda e, tt=tt: e.tensor_tensor(stat[:, tt, 5:6], stat[:, tt, 3:4], stat[:, tt, 4:5], ALU.subtract), r=[sk], w=[sk])
        P.op("dve", lambda e, tt=tt: e.tensor_scalar(stat[:, tt, 5:6], stat[:, tt, 5:6], LN_EPS, None, ALU.add), r=[sk], w=[sk])
        P.op("act", lambda e, tt=tt: e.sqrt(stat[:, tt, 7:8], stat[:, tt, 5:6]), r=[sk], w=[sk])
        P.op("dve", lambda e, tt=tt: e.reciprocal(stat[:, tt, 6:7], stat[:, tt, 7:8]), r=[sk], w=[sk])
        P.op("dve", lambda e, tt=tt: e.tensor_scalar(X[:, tt, :], X[:, tt, :], stat[:, tt, 2:3], stat[:, tt, 6:7], ALU.subtract, ALU.mult), r=[sk, xk], w=[xk])
        P.op("dve", lambda e, tt=tt: e.tensor_tensor(X[:, tt, :], X[:, tt, :], G[:], ALU.mult), r=[xk, tag + "G"], w=[xk])
        P.op("dve", lambda e, tt=tt: e.tensor_tensor(X[:, tt, :], X[:, tt, :], B[:], ALU.add), r=[xk, tag + "B"], w=[xk])
        if out_dram is not None:
            P.op("sp", lambda e, tt=tt: e.dma_start(out=out_dram[tt * 128:(tt + 1) * 128, :], in_=X[:, tt, :]), r=[xk], w=[("out", tt)], dma=True, lane=P.rr_lane("out", 4))


import os
STAGES = os.environ.get('STAGES', 'ABC')
LEVEL = int(os.environ.get('LEVEL', '9'))
def moe_stage(P, nc, X, PS, ident, rw, rb, wgu, bgu, wd, bd, lng, lnb, out_dram, E=32, NGU=12, ND=8):
    with ExitStack() as st:
        xT = sbt(nc, st, "xT", [128, 8, 2048], BF16)
        gates = sbt(nc, st, "gates", [128, NTT, E], F32)
        bgT = sbt(nc, st, "bgT", [128, 16, E], F32)
        bgs = sbt(nc, st, "bgs", [128, 8, E], F32)
        WGU = sbt(nc, st, "WGU", [128, NGU, 8, 256], BF16)
        WD = sbt(nc, st, "WD", [128, ND, 1024], BF16)

        NEXP = int(os.environ.get('NEXP', str(E)))
        n_gu = NEXP * 8
        gu_dma_next = [0]
        wd_dma_next = [0]

        def dma_gu(g):
            e_, fc = divmod(g, 8)
            s = g % NGU
            P.op("pool", lambda e: e.dma_start(out=WGU[:, s, :, :], in_=wgu[e_, :, fc * 256:(fc + 1) * 256].rearrange("(k p) n -> p k n", p=128)),
                 w=[("wgu", s)], dma=True, lane="wgu%d" % s)

        def dma_wd(g):
            e_, fc = divmod(g, 8)
            s = g % ND
            P.op("pool", lambda e: e.dma_start(out=WD[:, s, :], in_=wd[e_, fc * 128:(fc + 1) * 128, :]), w=[("wd", s)], dma=True, lane="wd%d" % s)

        if 'B' not in STAGES: n_gu = 0
        for g in range(min(NGU, n_gu)):
            dma_gu(g)
        gu_dma_next[0] = min(NGU, n_gu)
        for g in range(min(ND, n_gu)):
            dma_wd(g)
        wd_dma_next[0] = min(ND, n_gu)

        with ExitStack() as sa:
            bgu_raw = sbt(nc, sa, "bgu_raw", [E, 2048], F32)
            bd_sb = sbt(nc, sa, "bd_sb", [E, 1024], F32)
            rw_sb = sbt(nc, sa, "rw_sb", [128, 8, E], F32)
            rb_sb = sbt(nc, sa, "rb_sb", [128, E], F32)
            xTf = sbt(nc, sa, "xTf", [128, 8, 128], F32)
            lg = sbt(nc, sa, "lg", [128, E], F32)
            ex = sbt(nc, sa, "ex", [128, E], F32)
            mask = sbt(nc, sa, "mask", [128, E], F32)
            top8 = sbt(nc, sa, "top8", [128, 8], F32)
            sm = sbt(nc, sa, "sm", [128, 4], F32)
            gT = sbt(nc, sa, "gT", [E, 128], F32)
            if not os.environ.get('SKIPB'):
                P.op("sp", lambda e: e.dma_start(out=bgu_raw[:], in_=bgu), w=["bgu_raw"], dma=True)
                P.op("sp", lambda e: e.dma_start(out=bd_sb[:], in_=bd), w=["bd_sb"], dma=True)
                P.op("sp", lambda e: e.dma_start(out=rw_sb[:], in_=rw.rearrange("(k p) n -> p k n", p=128)), w=["rw_sb"], dma=True)
                P.op("sp", lambda e: e.dma_start(out=rb_sb[:], in_=rb.partition_broadcast(128)), w=["rb_sb"], dma=True)
                pb = PS[:, 6, 0:16 * E].rearrange("p (a e) -> p a e", e=E)
                for fc in range(8):
                    for j in range(2):
                        P.op("pe", lambda e, fc=fc, j=j: e.transpose(pb[:, fc * 2 + j, :], bgu_raw[:, fc * 256 + j:(fc + 1) * 256:2], ident[0:E, 0:E]),
                             r=["bgu_raw", "ident"], w=[("ps", 6)])
                P.op("act", lambda e: e.copy(bgT[:], pb), r=[("ps", 6)], w=["bgT"])
                bgT_g = bgT[:].rearrange("p (f j) e -> p f j e", j=2)[:, :, 0, :]
                P.op("dve", lambda e: e.tensor_scalar(bgs[:], bgT_g, 1.702, None, ALU.mult), r=["bgT"], w=["bgs"])
            ptr = PS[:, 0:2, :].rearrange("p b (k n) -> p (b k) n", n=128)
            plg = PS[:, 2, 0:E]
            pgT = PS[0:E, 3, 0:128]
            pbd = PS[:, 4:6, :]
            for tt in range(NTT):
                xk = ("X", tt)
                for k in range(8):
                    P.op("pe", lambda e, tt=tt, k=k: e.transpose(ptr[:, k, :], X[:, tt, k * 128:(k + 1) * 128], ident[:]),
                         r=[xk, "ident"], w=[("ps", k // 4)])
                P.op("act", lambda e, tt=tt: e.copy(xT[:, :, tt * 128:(tt + 1) * 128], ptr), r=[("ps", 0), ("ps", 1)], w=[("xT", tt // 4)])
                P.op("act", lambda e: e.copy(xTf[:], ptr), r=[("ps", 0), ("ps", 1)], w=["xTf"])
                if LEVEL < 2: continue
                for k in range(8):
                    P.op("pe", lambda e, k=k: e.matmul(plg, xTf[:, k, :], rw_sb[:, k, :], start=(k == 0), stop=(k == 7)),
                         r=["xTf", "rw_sb"], w=[("ps", 2)])
                P.op("dve", lambda e: e.tensor_tensor(lg[:], plg, rb_sb[:], ALU.add), r=[("ps", 2), "rb_sb"], w=["lg"])
                P.op("dve", lambda e: e.max(out=top8[:], in_=lg[:]), r=["lg"], w=["top8"])
                if LEVEL < 3: continue
                P.op("dve", lambda e: e.tensor_scalar(mask[:], lg[:], top8[:, 3:4], None, ALU.is_ge), r=["lg", "top8"], w=["mask"])
                P.op("dve", lambda e: e.tensor_scalar(sm[:, 0:1], top8[:, 0:1], -1.0, None, ALU.mult), r=["top8"], w=["sm0"])
                P.op("act", lambda e: e.activation(out=ex[:], in_=lg[:], func=AF.Exp, bias=sm[:, 0:1], scale=1.0), r=["lg", "sm0"], w=["ex"])
                P.op("dve", lambda e: e.tensor_tensor(ex[:], ex[:], mask[:], ALU.mult), r=["ex", "mask"], w=["ex"])
                P.op("dve", lambda e: e.reduce_sum(out=sm[:, 1:2], in_=ex[:], axis=AX.X), r=["ex"], w=["sm1"])
                P.op("dve", lambda e: e.reciprocal(sm[:, 2:3], sm[:, 1:2]), r=["sm1"], w=["sm2"])
                P.op("dve", lambda e, tt=tt: e.tensor_scalar(gates[:, tt, :], ex[:], sm[:, 2:3], None, ALU.mult), r=["ex", "sm2"], w=[("gates", tt)])
                if LEVEL < 4: continue
                P.op("pe", lambda e, tt=tt: e.transpose(pgT, gates[:, tt, :], ident[:]), r=[("gates", tt), "ident"], w=[("ps", 3)])
                P.op("act", lambda e: e.copy(gT[:], pgT), r=[("ps", 3)], w=["gT"])
                for h in range(2):
                    P.op("pe", lambda e, h=h: e.matmul(pbd[:, h, :], gT[:], bd_sb[:, h * 512:(h + 1) * 512], start=True, stop=True),
                         r=["gT", "bd_sb"], w=[("ps", 4 + h)])
                P.op("dve", lambda e, tt=tt: e.scalar_tensor_tensor(out=X[:, tt, :], in0=X[:, tt, :], scalar=ALPHA, in1=pbd.rearrange("p b n -> p (b n)"), op0=ALU.mult, op1=ALU.add),
                     r=[xk, ("ps", 4), ("ps", 5)], w=[xk])
            P.barrier()
            P.emit()

        with ExitStack() as sb_:
            AS = int(os.environ.get('ACTS', '1'))
            actT = sbt(nc, sb_, "actT", [128, 2, 8, 512 * AS], BF16)
            tg_ = sbt(nc, sb_, "tg_", [128, 2, 512], F32)
            ts_ = sbt(nc, sb_, "ts_", [128, 2, 512], F32)
            tl_ = sbt(nc, sb_, "tl_", [128, 2, 512], F32)
            cnt = [0]

            def GU(i):
                e_, tg = divmod(i, NTT // 4)
                par = i % 2
                for fc in range(8):
                    g = e_ * 8 + fc
                    s = g % NGU
                    q = cnt[0] % 2
                    cnt[0] += 1
                    bg_, bl_ = 0 + q, 2 + q
                    for j, bank in ((0, bg_), (1, bl_)):
                        for k in range(8):
                            P.op("pe", lambda e, s=s, k=k, j=j, bank=bank, tg=tg: e.matmul(PS[:, bank, :], WGU[:, s, k, j:256:2], xT[:, k, tg * 512:(tg + 1) * 512], start=(k == 0), stop=(k == 7)),
                                 r=[("wgu", s), ("xT", tg)], w=[("ps", bank)])
                    if tg == NTT // 4 - 1 and gu_dma_next[0] < n_gu:
                        dma_gu(gu_dma_next[0])
                        gu_dma_next[0] += 1
                    if os.environ.get('NOSW'): continue
                    if os.environ.get('SWONLY') != 'dve': P.op("act", lambda e, q=q, bank=bg_, fc=fc, e_=e_: e.activation(out=ts_[:, q, :], in_=PS[:, bank, :], func=AF.Sigmoid, bias=bgs[:, fc, e_:e_ + 1], scale=1.702),
                         r=[("ps", bg_), "bgs"], w=[("ts", q)])
                    if os.environ.get('SWONLY') not in ('act', 'dvelin'): P.op("dve", lambda e, q=q, bank=bg_, fc=fc, e_=e_: e.tensor_scalar(tg_[:, q, :], PS[:, bank, :], bgT[:, fc * 2, e_:e_ + 1], 7.0, ALU.add, ALU.min),
                         r=[("ps", bg_), "bgT"], w=[("tg", q)])
                    if os.environ.get('SWONLY') != 'act': P.op("dve", lambda e, q=q, bank=bl_, fc=fc, e_=e_: e.tensor_scalar(tl_[:, q, :], PS[:, bank, :], bgT[:, fc * 2 + 1, e_:e_ + 1], 7.0, ALU.add, ALU.min),
                         r=[("ps", bl_), "bgT"], w=[("tl", q)])
                    if os.environ.get('SWONLY') not in ('act', 'dvepsum'): P.op("dve", lambda e, q=q: e.tensor_scalar(tl_[:, q, :], tl_[:, q, :], -7.0, 1.0, ALU.max, ALU.add), r=[("tl", q)], w=[("tl", q)])
                    if os.environ.get('SWONLY') not in ('act', 'dvepsum'): P.op("dve", lambda e, q=q: e.tensor_tensor(tg_[:, q, :], tg_[:, q, :], ts_[:, q, :], ALU.mult), r=[("tg", q), ("ts", q)], w=[("tg", q)])
                    if os.environ.get('SWONLY') not in ('act', 'dvepsum'): P.op("dve", lambda e, q=q, par=par, fc=fc: e.tensor_tensor(actT[:, par, fc, 0:512 * AS:AS], tg_[:, q, :], tl_[:, q, :], ALU.mult),
                         r=[("tg", q), ("tl", q)], w=[("actT", par, fc)])

            def DP(i):
                e_, tg = divmod(i, NTT // 4)
                par = i % 2
                for tq in range(4):
                    tt = tg * 4 + tq
                    b0 = 4 + 2 * (tt % 2)
                    if os.environ.get('DPBANK'): b0 = int(os.environ['DPBANK'])
                    for h in range(2):
                        for fc in range(8):
                            g = e_ * 8 + fc
                            s = g % ND
                            P.op("pe", lambda e, h=h, fc=fc, s=s, par=par, tq=tq, b0=b0: e.matmul(PS[:, b0 + h, :], (xT[:, fc, tq * 128:(tq + 1) * 128] if os.environ.get('DPXT') else actT[:, par, fc, tq * 128 * AS:(tq + 1) * 128 * AS:AS]), WD[:, s, h * 512:(h + 1) * 512], start=(fc == 0), stop=(fc == 7)),
                                 r=([("wd", s)] if os.environ.get('DPXT') == '2' else [("actT", par, fc), ("wd", s)]), w=[("ps", b0 + h)])
                    if tt == NTT - 1:
                        for fc in range(8):
                            if wd_dma_next[0] < n_gu:
                                dma_wd(wd_dma_next[0])
                                wd_dma_next[0] += 1
                    if os.environ.get('NOSTT'): continue
                    P.op("dve", lambda e, tt=tt, e_=e_, b0=b0: e.scalar_tensor_tensor(out=X[:, tt, :], in0=PS[:, b0:b0 + 2, :].rearrange("p b n -> p (b n)"), scalar=gates[:, tt, e_:e_ + 1], in1=X[:, tt, :], op0=ALU.mult, op1=ALU.add),
                         r=[("ps", b0), ("ps", b0 + 1), ("gates", tt), ("X", tt)], w=[("X", tt)])

            n_items = NEXP * (NTT // 4) if 'B' in STAGES else 0
            for i in range(n_items + 1):
                if i < n_items:
                    GU(i)
                if i >= 1 and not os.environ.get('NODP') and (i - 1) < int(os.environ.get('DPMAX', '99999')):
                    DP(i - 1)
            P.barrier()
            P.emit()

        with ExitStack() as sc:
            if 'C' in STAGES:
                layer_norm_out(P, nc, sc, X, lng, lnb, out_dram, "lnf")
            else:
                for tt in range(NTT):
                    P.op("sp", lambda e, tt=tt: e.dma_start(out=out_dram[tt * 128:(tt + 1) * 128, :], in_=X[:, tt, :]), r=[("X", tt)], w=[("out", tt)], dma=True, lane=P.rr_lane("out", 4))
            P.barrier()
            P.emit()


def build_moe_prog(E=32):
    nc = bass.Bass("TRN2", target_bir_lowering=False)
    dt = lambda name, shape, kind="ExternalInput": nc.dram_tensor(name, shape, F32, kind=kind).ap()
    x = dt("x", [NTT * 128, 1024])
    identd = dt("ident", [128, 128])
    rw = dt("rw", [1024, E]); rb = dt("rb", [1, E])
    EE = E if 'B' in STAGES else 1
    wgu = dt("wgu", [EE, 1024, 2048]); bgu = dt("bgu", [E, 2048])
    wd = dt("wd", [EE, 1024, 1024]); bd = dt("bd", [E, 1024])
    lng = dt("lng", [1, 1024]); lnb = dt("lnb", [1, 1024])
    y = dt("y", [NTT * 128, 1024], "ExternalOutput")
    with ExitStack() as st:
        P = Prog(nc, st)
        X = sbt(nc, st, "X", [128, NTT, 1024], F32)
        ident = sbt(nc, st, "identsb", [128, 128], F32)
        PS = st.enter_context(nc.psum_tensor("PS", [128, 8, 512], F32))
        P.op("sp", lambda e: e.dma_start(out=ident[:], in_=identd), w=["ident"], dma=True)
        for tt in range(NTT):
            P.op("sp", lambda e, tt=tt: e.dma_start(out=X[:, tt, :], in_=x[tt * 128:(tt + 1) * 128, :]), w=[("X", tt)], dma=True, lane=P.rr_lane("in", 8))
        moe_stage(P, nc, X, PS, ident, rw, rb, wgu, bgu, wd, bd, lng, lnb, y, E=E)
        print("total ops", P.n_total)
    return nc
from contextlib import ExitStack

BIG = 30000.0
PI = math.pi


def attn_stage(P, nc, X, PS, ident, xkv, pos, wqkv, wo, ropec, pastmask, causal, lng, lnb, out_dram, NTK=32, NTO=16, NH=16, xstack=None):
    TK = NTK * 128
    TO = NTO * 128
    NB = NTK // 2
    NBP = (NTK - NTO) // 2
    own0 = TK - TO
    PSb = PS[:, 2, :].bitcast(BF16)
    with ExitStack() as st:
        O = sbt(nc, st, "a_O", [128, NTO, 1024], BF16)
        rc = sbt(nc, st, "a_rc", [128, 8], F32)
        pm = sbt(nc, st, "a_pm", [128, NTO, NB], F32)
        cm = sbt(nc, st, "a_cm", [128, 2, 256], F32)
        identb = sbt(nc, st, "a_identb", [128, 128], BF16)
        s1 = ExitStack()
        xT = sbt(nc, s1, "a_xT", [128, 8, TK], BF16)
        CS = sbt(nc, s1, "a_CS", [64, 2, TK], BF16)
        P.op("sp", lambda e: e.dma_start(out=rc[:], in_=ropec), w=["rc"], dma=True)
        P.op("sp", lambda e: e.dma_start(out=pm[:], in_=pastmask.rearrange("p (t n) -> p t n", n=NB)), w=["pm"], dma=True)
        P.op("sp", lambda e: e.dma_start(out=cm[:], in_=causal.rearrange("p (t n) -> p t n", n=256)), w=["cm"], dma=True)
        P.op("dve", lambda e: e.tensor_copy(identb[:], ident[:]), r=["ident"], w=["identb"])
        if NH < 16:
            P.op("pool", lambda e: e.memset(O[:], 0.0), w=[("O", t) for t in range(NTO)])
        with ExitStack() as sa:
            RC = min(1024, TK)
            posi = sbt(nc, sa, "a_posi", [64, RC], I32)
            ang = sbt(nc, sa, "a_ang", [64, RC], F32)
            m1 = sbt(nc, sa, "a_m1", [64, RC], F32)
            xtmp = sbt(nc, sa, "a_xtmp", [128, 2, 1024], F32)
            ti = posi
            def fold(dst):
                P.op("dve", lambda e: e.tensor_scalar(m1[:], dst[:], PI, -2 * PI, ALU.is_gt, ALU.mult), r=["ang"], w=["m1"])
                P.op("dve", lambda e: e.tensor_tensor(dst[:], dst[:], m1[:], ALU.add), r=["ang", "m1"], w=["ang"])
                P.op("dve", lambda e: e.tensor_scalar(m1[:], dst[:], -PI, 2 * PI, ALU.is_lt, ALU.mult), r=["ang"], w=["m1"])
                P.op("dve", lambda e: e.tensor_tensor(dst[:], dst[:], m1[:], ALU.add), r=["ang", "m1"], w=["ang"])
            for rcn in range(TK // RC):
                cs_ = slice(rcn * RC, (rcn + 1) * RC)
                P.op("sp", lambda e, cs_=cs_: e.dma_start(out=posi[:], in_=pos[:, cs_].partition_broadcast(64)), w=["posi"], dma=True)
                P.op("dve", lambda e: e.tensor_copy(ang[:], posi[:]), r=["posi"], w=["ang"])
                P.op("dve", lambda e: e.tensor_scalar(ang[:], ang[:], rc[0:64, 0:1], None, ALU.mult), r=["ang", "rc"], w=["ang"])
                P.op("dve", lambda e: e.tensor_scalar(m1[:], ang[:], 1.0 / (2 * PI), None, ALU.mult), r=["ang"], w=["m1"])
                P.op("dve", lambda e: e.tensor_copy(ti[:], m1[:]), r=["m1", "ang"], w=["posi"])
                P.op("dve", lambda e: e.tensor_copy(m1[:], ti[:]), r=["posi"], w=["m1"])
                P.op("dve", lambda e: e.scalar_tensor_tensor(out=ang[:], in0=m1[:], scalar=-2 * PI, in1=ang[:], op0=ALU.mult, op1=ALU.add), r=["m1", "ang"], w=["ang"])
                fold(ang)
                P.op("act", lambda e, cs_=cs_: e.activation(out=CS[:, 1, cs_], in_=ang[:], func=AF.Sin), r=["ang"], w=["CS1"])
                P.op("dve", lambda e, cs_=cs_: e.tensor_scalar(CS[:, 1, cs_], CS[:, 1, cs_], rc[0:64, 1:2], None, ALU.mult), r=["CS1", "rc"], w=["CS1"])
                P.op("dve", lambda e: e.tensor_scalar(ang[:], ang[:], 0.5 * PI, None, ALU.add), r=["ang", "CS1"], w=["ang"])
                fold(ang)
                P.op("act", lambda e, cs_=cs_: e.activation(out=CS[:, 0, cs_], in_=ang[:], func=AF.Sin), r=["ang"], w=["CS0"])
                P.op("dve", lambda e, cs_=cs_: e.tensor_scalar(CS[:, 0, cs_], CS[:, 0, cs_], rc[0:64, 2:3], rc[0:64, 3:4], ALU.mult, ALU.add), r=["CS0", "rc"], w=["CS0"])
            ptr = PS[:, 0:2, :].rearrange("p b (k n) -> p (b k) n", n=128)
            for tt in range(NTK):
                q = tt % 2
                P.op("sp", lambda e, tt=tt, q=q: e.dma_start(out=xtmp[:, q, :], in_=xkv[tt * 128:(tt + 1) * 128, :]), w=[("xtmp", q)], dma=True, lane="ax%d" % q)
                for k in range(8):
                    P.op("pe", lambda e, q=q, k=k: e.transpose(ptr[:, k, :], xtmp[:, q, k * 128:(k + 1) * 128], ident[:]), r=[("xtmp", q), "ident"], w=[("ps", k // 4)])
                P.op("act", lambda e, tt=tt: e.copy(xT[:, :, tt * 128:(tt + 1) * 128], ptr), r=[("ps", 0), ("ps", 1)], w=[("xT", tt)])
            P.barrier(); P.emit()
        xTk = [("xT", tt) for tt in range(NTK)]
        with ExitStack() as sh:
            Wh = sbt(nc, sh, "a_Wh", [128, 8, 192], BF16)
            Wr = sbt(nc, sh, "a_Wr", [128, 8, 128], BF16)
            QT = sbt(nc, sh, "a_QT", [64, TO], BF16)
            KT = sbt(nc, sh, "a_KT", [64, TK], BF16)
            V = sbt(nc, sh, "a_V", [128, NTK, 64], BF16)
            t1 = sbt(nc, sh, "a_t1", [64, 2, 512], F32)
            t2 = sbt(nc, sh, "a_t2", [64, 2, 512], F32)
            ksum = sbt(nc, sh, "a_ksum", [64, NB], F32)
            kmb = sbt(nc, sh, "a_kmb", [64, NB], BF16)
            gm = sbt(nc, sh, "a_gm", [128, NTO, NB], F32)
            top8 = sbt(nc, sh, "a_top8", [128, NTO, 8], F32)
            thr = sbt(nc, sh, "a_thr", [128, NTO], F32)
            bias = sbt(nc, sh, "a_bias", [128, NTO, NB], F32)
            Qsq = sbt(nc, sh, "a_Qsq", [64, TO], F32)
            Ksq = sbt(nc, sh, "a_Ksq", [64, TK], F32)
            onesc = sbt(nc, sh, "a_onesc", [128, 128], F32)
            kmx = sbt(nc, sh, "a_kmx", [1, 16], F32)
            kb = sbt(nc, sh, "a_kb", [128, 2], F32)
            Mq = sbt(nc, sh, "a_Mq", [128, 2, NTO], F32)
            dparts = sbt(nc, sh, "a_dparts", [128, NTO, NB + 1], F32)
            otmp = sbt(nc, sh, "a_otmp", [128, 2, 256], F32)
            Pb = sbt(nc, sh, "a_Pb", [128, 2, TK], BF16)
            PT = sbt(nc, sh, "a_PT", [128, 2, 8, 128], BF16)
            sm = sbt(nc, sh, "a_sm", [128, NTO, 4], F32)
            P.op("pool", lambda e: e.memset(Wr[:], 0.0), w=["Wr"])
            P.op("dve", lambda e: e.memset(onesc[:], 1.0), w=["onesc"])
            ptc = [0]
            evc = [0]
            pjc = [0]
            for h in range(NH):
                for j, c0 in enumerate((h * 64, 1024 + h * 64, 2048 + h * 64)):
                    P.op("pool", lambda e, j=j, c0=c0: e.dma_start(out=Wh[:, :, j * 64:(j + 1) * 64], in_=wqkv[:, c0:c0 + 64].rearrange("(k p) n -> p k n", p=128)),
                         w=[("Wh", j)], dma=True, lane="wh%d" % j)
                for j in range(2):
                    P.op("act", lambda e, j=j: e.copy(Wr[:, :, j * 64:j * 64 + 8], Wh[:, :, j * 64 + 8:j * 64 + 16]), r=[("Wh", j)], w=["Wr"])
                    P.op("act", lambda e, j=j: e.copy(Wr[:, :, j * 64 + 8:j * 64 + 16], Wh[:, :, j * 64:j * 64 + 8]), r=[("Wh", j)], w=["Wr"])
                for (j, dst, tok0, nch, scl, dk) in ((0, QT, own0, TO // 512, 0.125, "QT"), (1, KT, 0, TK // 512, 1.0, "KT")):
                    for c in range(nch):
                        a0 = tok0 + c * 512
                        pj = pjc[0] % 2
                        pjc[0] += 1
                        b0_, b1_ = 2 * pj, 2 * pj + 1
                        for (bank, Wt) in ((b0_, Wh), (b1_, Wr)):
                            for k in range(8):
                                P.op("pe", lambda e, bank=bank, Wt=Wt, k=k, j=j, a0=a0: e.matmul(PS[0:64, bank, :], Wt[:, k, j * 64:(j + 1) * 64], xT[:, k, a0:a0 + 512], start=(k == 0), stop=(k == 7)),
                                     r=[("Wh", j), "Wr"] + xTk[a0 // 128:a0 // 128 + 4], w=[("ps", bank)])
                        P.op("dve", lambda e, a0=a0, pj=pj, b0_=b0_: e.tensor_tensor(t1[:, pj, :], PS[0:64, b0_, :], CS[:, 0, a0:a0 + 512], ALU.mult), r=[("ps", b0_), "CS0"], w=[("t1", pj)])
                        P.op("dve", lambda e, a0=a0, scl=scl, pj=pj, b1_=b1_: e.scalar_tensor_tensor(out=t2[:, pj, :], in0=PS[0:64, b1_, :], scalar=scl, in1=CS[:, 1, a0:a0 + 512], op0=ALU.mult, op1=ALU.mult), r=[("ps", b1_), "CS1"], w=[("t2", pj)])
                        P.op("dve", lambda e, dst=dst, c=c, scl=scl, pj=pj: e.scalar_tensor_tensor(out=dst[:, c * 512:(c + 1) * 512], in0=t1[:, pj, :], scalar=scl, in1=t2[:, pj, :], op0=ALU.mult, op1=ALU.add), r=[("t1", pj), ("t2", pj)], w=[(dk, c)])
                QTk = [("QT", c) for c in range(TO // 512)]
                KTk = [("KT", c) for c in range(TK // 512)]
                for g8 in range(NTK // 8):
                    pv = PS[:, 2, :].rearrange("p (t d) -> p t d", d=64)
                    for i in range(8):
                        tt = g8 * 8 + i
                        for k in range(8):
                            P.op("pe", lambda e, i=i, tt=tt, k=k: e.matmul(pv[:, i, :], xT[:, k, tt * 128:(tt + 1) * 128], Wh[:, k, 128:192], start=(k == 0), stop=(k == 7)),
                                 r=[("Wh", 2), ("xT", tt)], w=[("ps", 2)])
                    P.op("act", lambda e, g8=g8: e.copy(V[:, g8 * 8:(g8 + 1) * 8, :], pv), r=[("ps", 2)], w=[("V", g8)])
                Vk = [("V", g8) for g8 in range(NTK // 8)]
                P.op("dve", lambda e: e.tensor_reduce(out=ksum[:], in_=KT[:].rearrange("p (n k) -> p n k", k=256), axis=AX.X, op=ALU.add), r=KTk, w=["ksum"])
                P.op("dve", lambda e: e.tensor_copy(kmb[:], ksum[:]), r=["ksum"], w=["kmb"])
                pg = PS[:, 3, 0:NTO * NB].rearrange("p (t n) -> p t n", n=NB)
                for t in range(NTO):
                    P.op("pe", lambda e, t=t: e.matmul(pg[:, t, :], QT[:, t * 128:(t + 1) * 128], kmb[:], start=True, stop=True), r=[("QT", t // 4), "kmb"], w=[("ps", 3)])
                P.op("dve", lambda e: e.tensor_tensor(gm[:], pg, pm[:], ALU.add), r=[("ps", 3), "pm"], w=["gm"])
                for t in range(NTO):
                    P.op("dve", lambda e, t=t: e.max(out=top8[:, t, :], in_=gm[:, t, :]), r=["gm"], w=["top8"])
                P.op("dve", lambda e: e.tensor_scalar(thr[:], top8[:, :, 2], -1e29, None, ALU.max), r=["top8"], w=["thr"])
                for t in range(NTO):
                    P.op("dve", lambda e, t=t: e.tensor_scalar(bias[:, t, :], gm[:, t, :], thr[:, t:t + 1], 1.0, ALU.is_ge, ALU.subtract), r=["gm", "thr"], w=["bias"])
                P.op("dve", lambda e: e.tensor_scalar(bias[:], bias[:], BIG, None, ALU.mult), r=["bias"], w=["bias"])
                P.op("dve", lambda e: e.tensor_tensor(Qsq[:], QT[:], QT[:], ALU.mult), r=QTk, w=["Qsq"])
                P.op("dve", lambda e: e.tensor_tensor(Ksq[:], KT[:], KT[:], ALU.mult), r=KTk, w=["Ksq"])
                pn = PS[:, 3, 256:256 + NTO]
                for t in range(NTO):
                    P.op("pe", lambda e, t=t: e.matmul(PS[:, 3, 256 + t:257 + t], Qsq[:, t * 128:(t + 1) * 128], onesc[0:64, 0:1], start=True, stop=True), r=["Qsq", "onesc"], w=[("ps", 3)])
                for c in range(TK // 512):
                    P.op("pe", lambda e, c=c: e.matmul(PS[0:1, c % 2, :], onesc[0:64, 0:1], Ksq[:, c * 512:(c + 1) * 512], start=True, stop=True), r=["Ksq", "onesc"], w=[("ps", c % 2)])
                    P.op("dve", lambda e, c=c: e.reduce_max(out=kmx[0:1, c:c + 1], in_=PS[0:1, c % 2, :], axis=AX.X), r=[("ps", c % 2)], w=["kmx"])
                P.op("dve", lambda e: e.reduce_max(out=kmx[0:1, 15:16], in_=kmx[0:1, 0:TK // 512], axis=AX.X), r=["kmx"], w=["kmx"])
                P.op("pe", lambda e: e.matmul(PS[:, 3, 300:301], onesc[0:1, 0:128], kmx[0:1, 15:16], start=True, stop=True), r=["kmx", "onesc"], w=[("ps", 3)])
                P.op("act", lambda e: e.copy(kb[:, 0:1], PS[:, 3, 300:301]), r=[("ps", 3)], w=["kb"])
                P.op("dve", lambda e: e.tensor_scalar(Mq[:, 0, :], pn, kb[:, 0:1], 1.0201, ALU.mult, ALU.mult), r=[("ps", 3), "kb"], w=["Mq"])
                P.op("act", lambda e: e.sqrt(Mq[:, 1, :], Mq[:, 0, :]), r=["Mq"], w=["Mq"])
                P.op("dve", lambda e: e.tensor_scalar(Mq[:, 1, :], Mq[:, 1, :], -1.0, None, ALU.mult), r=["Mq"], w=["Mq"])
                for t in range(NTO):
                    P.op("dve", lambda e, t=t: e.tensor_scalar(bias[:, t, :], bias[:, t, :], Mq[:, 1, t:t + 1], None, ALU.add), r=["bias", "Mq"], w=["bias"])
                def S_phase(t, h=h):
                    par = t % 2
                    gj = NBP + t // 2
                    nblk = gj + 1
                    for n0 in range(0, nblk, 2):
                        nn = min(2, nblk - n0)
                        evc[0] += 1
                        bank = evc[0] % 2
                        P.op("pe", lambda e, n0=n0, nn=nn, t=t, bank=bank: e.matmul(PS[:, bank, 0:nn * 256], QT[:, t * 128:(t + 1) * 128], KT[:, n0 * 256:(n0 + nn) * 256], start=True, stop=True),
                             r=[("QT", t // 4), ("KT", n0 // 2), ("KT", (n0 + nn - 1) // 2)], w=[("ps", bank)])
                        for i in range(nn):
                            n = n0 + i
                            if n < gj:
                                P.op("act", lambda e, i=i, n=n, t=t, par=par, bank=bank: e.activation(out=Pb[:, par, n * 256:(n + 1) * 256], in_=PS[:, bank, i * 256:(i + 1) * 256], func=AF.Exp, bias=bias[:, t, n:n + 1], scale=1.0, accum_out=dparts[:, t, n:n + 1]),
                                     r=[("ps", bank), "bias"], w=[("Pb", par), ("dp", t)])
                            else:
                                oq = t % 2
                                P.op("dve", lambda e, i=i, t=t, oq=oq, bank=bank: e.tensor_tensor(otmp[:, oq, :], PS[:, bank, i * 256:(i + 1) * 256], cm[:, t % 2, :], ALU.add),
                                     r=[("ps", bank), "cm"], w=[("otmp", oq)])
                                P.op("act", lambda e, n=n, t=t, par=par, oq=oq: e.activation(out=Pb[:, par, n * 256:(n + 1) * 256], in_=otmp[:, oq, :], func=AF.Exp, bias=Mq[:, 1, t:t + 1], scale=1.0, accum_out=dparts[:, t, n:n + 1]),
                                     r=[("otmp", oq), "Mq"], w=[("Pb", par), ("dp", t)])
                    P.op("dve", lambda e, t=t, nblk=nblk: e.reduce_sum(out=sm[:, t, 2:3], in_=dparts[:, t, 0:nblk], axis=AX.X), r=[("dp", t)], w=[("sm", t)])

                def T_phase(t, h=h):
                    par = t % 2
                    gj = NBP + t // 2
                    nblk = gj + 1
                    nkt = 2 * nblk
                    po = PS[:, 3, 512 - 64:512]
                    for k0 in range(0, nkt, 8):
                        slot = ptc[0] % 2
                        ptc[0] += 1
                        tb = 2
                        PSx = PS[:, tb, :].bitcast(BF16)
                        ni = min(8, nkt - k0)
                        for i in range(ni):
                            kt = k0 + i
                            P.op("pe", lambda e, i=i, kt=kt, par=par, PSx=PSx: e.transpose(PSx[:, i * 128:(i + 1) * 128], Pb[:, par, kt * 128:(kt + 1) * 128], identb[:]), r=[("Pb", par), "identb"], w=[("ps", tb)])
                        if False:
                            P.op("act", lambda e, slot=slot, ni=ni, PSx=PSx: e.copy(PT[:, slot, 0:ni, :], PSx[:, 0:ni * 128].rearrange("p (i n) -> p i n", n=128)), r=[("ps", tb)], w=[("PT", slot)])
                        else:
                            P.op("dve", lambda e, slot=slot, ni=ni, PSx=PSx: e.tensor_copy(PT[:, slot, 0:ni, :], PSx[:, 0:ni * 128].rearrange("p (i n) -> p i n", n=128)), r=[("ps", tb)], w=[("PT", slot)])
                        for i in range(ni):
                            kt = k0 + i
                            P.op("pe", lambda e, i=i, kt=kt, slot=slot, nkt=nkt: e.matmul(po, PT[:, slot, i, :], V[:, kt, :], start=(kt == 0), stop=(kt == nkt - 1)),
                                 r=[("PT", slot), ("V", kt // 8)], w=[("ps", 3)])
                    P.op("dve", lambda e, t=t: e.reciprocal(sm[:, t, 3:4], sm[:, t, 2:3]), r=[("sm", t)], w=[("sm", t)])
                    P.op("dve", lambda e, t=t, h=h: e.tensor_scalar(O[:, t, h * 64:(h + 1) * 64], po, sm[:, t, 3:4], None, ALU.mult), r=[("ps", 3), ("sm", t)], w=[("O", t)])
                for t in range(NTO + 1):
                    if os.environ.get('SKIPATT'): break
                    if t < NTO and not os.environ.get('SKIPS'):
                        S_phase(t)
                    if t >= 1 and not os.environ.get('SKIPT'):
                        T_phase(t - 1)
            P.barrier(); P.emit()
        s1.close()
        with ExitStack() as so:
            if X is None:
                if xstack is not None:
                    X = sbt(nc, xstack, "Xres", [128, NTO, 1024], F32, side="right")
                else:
                    X = sbt(nc, so, "a_X", [128, NTO, 1024], F32)
            Wo = sbt(nc, so, "a_Wo", [128, 8, 1024], BF16)
            OT = sbt(nc, so, "a_OT", [128, 8, 128], BF16)
            xtmp2 = sbt(nc, so, "a_xtmp2", [128, 2, 1024], F32)
            for c in range(8):
                P.op("pool", lambda e, c=c: e.dma_start(out=Wo[:, c, :], in_=wo[c * 128:(c + 1) * 128, :]), w=[("Wo", c)], dma=True, lane="wo%d" % (c % 4))
            for t in range(NTO):
                q = t % 2
                P.op("sp", lambda e, t=t, q=q: e.dma_start(out=xtmp2[:, q, :], in_=xkv[own0 + t * 128:own0 + (t + 1) * 128, :]), w=[("xtmp2", q)], dma=True, lane="ax%d" % q)
                for c in range(8):
                    P.op("pe", lambda e, t=t, c=c: e.transpose(PSb[:, c * 128:(c + 1) * 128], O[:, t, c * 128:(c + 1) * 128], identb[:]), r=[("O", t), "identb"], w=[("ps", 2)])
                P.op("act", lambda e: e.copy(OT[:], PSb[:].rearrange("p (c n) -> p c n", n=128)), r=[("ps", 2)], w=["OT"])
                for hf in range(2):
                    for c in range(8):
                        P.op("pe", lambda e, hf=hf, c=c: e.matmul(PS[:, hf, :], OT[:, c, :], Wo[:, c, hf * 512:(hf + 1) * 512], start=(c == 0), stop=(c == 7)),
                             r=["OT", ("Wo", c)], w=[("ps", hf)])
                P.op("dve", lambda e, t=t, q=q: e.scalar_tensor_tensor(out=X[:, t, :], in0=xtmp2[:, q, :], scalar=ALPHA, in1=PS[:, 0:2, :].rearrange("p b n -> p (b n)"), op0=ALU.mult, op1=ALU.add),
                     r=[("xtmp2", q), ("ps", 0), ("ps", 1)], w=[("X", t)])
            P.barrier(); P.emit()
            if lng is not None:
                with ExitStack() as sc:
                    layer_norm_out(P, nc, sc, X, lng, lnb, out_dram, "lna")
                    P.barrier(); P.emit()
    return X


def rope_consts():
    import numpy as np
    rc = np.zeros((128, 8), np.float32)
    for p in range(64):
        if p < 16:
            rc[p, 0] = 500000.0 ** (-(2 * (p % 8)) / 16.0)
            rc[p, 1] = -1.0 if p < 8 else 1.0
            rc[p, 2] = 1.0
            rc[p, 3] = 0.0
        else:
            rc[p, 0] = 0.0; rc[p, 1] = 0.0; rc[p, 2] = 0.0; rc[p, 3] = 1.0
    rc[:, 4] = -math.pi
    return rc


def attn_masks(NTK, NTO, has_prev):
    import numpy as np
    NB = NTK // 2; NBP = (NTK - NTO) // 2
    pm = np.full((NTO, NB), -1e30, np.float32)
    for t in range(NTO):
        gj = NBP + t // 2
        for n in range(NB):
            if n < gj and (has_prev or n >= NBP):
                pm[t, n] = 0.0
    pm = np.broadcast_to(pm.reshape(1, -1), (128, NTO * NB)).copy()
    cm = np.zeros((128, 2, 256), np.float32)
    for par in range(2):
        qpos = par * 128 + np.arange(128)[:, None]
        kpos = np.arange(256)[None, :]
        cm[:, par, :] = np.where(kpos <= qpos, 0.0, -BIG)
    return pm, cm.reshape(128, 512)


def build_attn_prog(NTK=32, NTO=16, NH=16, ln=True):
    nc = bass.Bass("TRN2", target_bir_lowering=False)
    dt = lambda name, shape, kind="ExternalInput", d=F32: nc.dram_tensor(name, shape, d, kind=kind).ap()
    xkv = dt("xkv", [NTK * 128, 1024]); pos = dt("pos", [1, NTK * 128], d=I32)
    identd = dt("ident", [128, 128])
    wqkv = dt("wqkv", [1024, 3072]); wo = dt("wo", [1024, 1024])
    ropec = dt("ropec", [128, 8]); pastmask = dt("pastmask", [128, NTO * (NTK // 2)]); causal = dt("causal", [128, 512])
    lng = dt("lng", [1, 1024]); lnb = dt("lnb", [1, 1024])
    y = dt("y", [NTO * 128, 1024], "ExternalOutput")
    with ExitStack() as st:
        P = Prog(nc, st)
        ident = sbt(nc, st, "identsb", [128, 128], F32)
        PS = st.enter_context(nc.psum_tensor("PS", [128, 8, 512], F32))
        P.op("sp", lambda e: e.dma_start(out=ident[:], in_=identd), w=["ident"], dma=True)
        attn_stage(P, nc, None, PS, ident, xkv, pos, wqkv, wo, ropec, pastmask, causal, lng if ln else None, lnb, y, NTK=NTK, NTO=NTO, NH=NH)
        if not ln:
            for tt in range(NTO):
                P.op("sp", lambda e, tt=tt: e.dma_start(out=y[tt * 128:(tt + 1) * 128, :], in_=X[:, tt, :]), r=[("X", tt)], w=[("out", tt)], dma=True, lane=P.rr_lane("out", 4))
            P.barrier(); P.emit()
        print("total ops", P.n_total)
    return nc
from contextlib import ExitStack

LN_EPS = 1e-5
KSCALE = 128 ** -0.5


def mlstm_stage(P, nc, X, PS, ident, xkv, w_in, b_gates, norm_g, w_out, consts, flag, lng, lnb, out_dram, NTK=32, NTO=16, x_in_sbuf=False, xkv_keys=(), prev_src=None):
    NTP = NTK - NTO
    PSb = PS[:, 2, :].bitcast(BF16)
    with ExitStack() as st:
        W = sbt(nc, st, "m_W", [128, 8, 3080], BF16)
        Wo = sbt(nc, st, "m_Wo", [128, 8, 1024], BF16)
        CN = sbt(nc, st, "m_CN", [128, 1280], F32)
        Tri = CN[:, 0:128]; I4 = CN[:, 128:640].rearrange("p (h t) -> p h t", t=128); Tri4 = CN[:, 640:1152].rearrange("p (h t) -> p h t", t=128); ones = CN[:, 1152:1280]
        Cst = sbt(nc, st, "m_C", [128, 4, 256], F32)
        nst = sbt(nc, st, "m_n", [128, 4], F32)
        car = sbt(nc, st, "m_car", [128, 2, 4], F32)
        bg = sbt(nc, st, "m_bg", [128, 8], F32)
        ng = sbt(nc, st, "m_ng", [128, 1024], F32)
        flg = sbt(nc, st, "m_flag", [128, 1], F32)
        identb = sbt(nc, st, "m_identb", [128, 128], BF16)
        xtmp = sbt(nc, st, "m_xtmp", [128, 1024], F32)
        xTc = sbt(nc, st, "m_xTc", [128, 8, 128], BF16)
        ktm = sbt(nc, st, "m_ktm", [128, 4, 128], F32)
        v = sbt(nc, st, "m_v", [128, 4, 256], F32)
        og = sbt(nc, st, "m_og", [128, 1024], F32)
        qT = sbt(nc, st, "m_qT", [128, 4, 128], F32)
        kT = sbt(nc, st, "m_kT", [128, 4, 128], F32)
        g = sbt(nc, st, "m_g", [128, 8, 4], F32)
        abc = sbt(nc, st, "m_abc", [128, 4, 128], F32)
        arep = sbt(nc, st, "m_arep", [128, 4, 128], F32)
        Mrep = sbt(nc, st, "m_Mrep", [128, 4, 128], F32)
        WT = sbt(nc, st, "m_WT", [128, 4, 128], F32)
        AT = sbt(nc, st, "m_AT", [128, 4, 128], F32)
        qp = sbt(nc, st, "m_qp", [128, 4, 128], F32)
        hh = sbt(nc, st, "m_h", [128, 4, 256], F32)
        hsq = sbt(nc, st, "m_hsq", [128, 4, 256], F32)
        hg = sbt(nc, st, "m_hg", [128, 1024], BF16)
        hgT = sbt(nc, st, "m_hgT", [128, 8, 128], BF16)
        ks = sbt(nc, st, "m_ks", [128, 4, 128], F32)
        s8 = sbt(nc, st, "m_s8", [128, 8, 4], F32)
        for k in range(8):
            P.op("pool", lambda e, k=k: e.dma_start(out=W[:, k, :], in_=w_in[k * 128:(k + 1) * 128, :]), w=[("W", k)], dma=True, lane="mw%d" % (k % 4))
        for k in range(8):
            P.op("pool", lambda e, k=k: e.dma_start(out=Wo[:, k, :], in_=w_out[k * 128:(k + 1) * 128, :]), w=[("Wo", k)], dma=True, lane="mw%d" % (k % 4))
        Wk = [("W", k) for k in range(8)]
        Wok = [("Wo", k) for k in range(8)]
        P.op("sp", lambda e: e.dma_start(out=CN[:], in_=consts), w=["CN"], dma=True)
        P.op("sp", lambda e: e.dma_start(out=bg[:], in_=b_gates.partition_broadcast(128)), w=["bg"], dma=True)
        P.op("sp", lambda e: e.dma_start(out=ng[:], in_=norm_g.partition_broadcast(128)), w=["ng"], dma=True)
        P.op("sp", lambda e: e.dma_start(out=flg[:], in_=flag), w=["flag"], dma=True)
        P.op("dve", lambda e: e.tensor_copy(identb[:], ident[:]), r=["ident"], w=["identb"])
        P.op("dve", lambda e: e.memset(Cst[:], 0.0), w=["C"])
        P.op("dve", lambda e: e.memset(nst[:], 0.0), w=["n"])
        P.op("dve", lambda e: e.memset(car[:], 0.0), w=["car"])
        ptr = PS[:, 0:2, :].rearrange("p b (k n) -> p (b k) n", n=128)
        for c in range(NTK):
            own = c >= NTP
            co = c - NTP
            if own:
                xsrc = X[:, co, :]
                xkey = ("X", co)
                if not x_in_sbuf:
                    P.op("sp", lambda e, c=c, co=co: e.dma_start(out=X[:, co, :], in_=xkv[c * 128:(c + 1) * 128, :]), w=[xkey], dma=True, lane=P.rr_lane("mx", 2))
            else:
                xsrc = xtmp[:]
                xkey = "xtmp"
                src_ap = prev_src(c) if prev_src is not None else xkv[c * 128:(c + 1) * 128, :]
                P.op("sp", lambda e, src_ap=src_ap: e.dma_start(out=xtmp[:], in_=src_ap), r=list(xkv_keys), w=[xkey], dma=True, lane=P.rr_lane("mx", 2))
            for k in range(8):
                P.op("pe", lambda e, k=k, xsrc=xsrc: e.transpose(ptr[:, k, :], xsrc[:, k * 128:(k + 1) * 128], ident[:]), r=[xkey, "ident"], w=[("ps", k // 4)])
            P.op("act", lambda e: e.copy(xTc[:], ptr), r=[("ps", 0), ("ps", 1)], w=["xTc"])
            def tm_proj(bank, c0, n):
                for k in range(8):
                    P.op("pe", lambda e, k=k, bank=bank, c0=c0, n=n: e.matmul(PS[:, bank, 0:n], xTc[:, k, :], W[:, k, c0:c0 + n], start=(k == 0), stop=(k == 7)),
                         r=["xTc", ("W", k)], w=[("ps", bank)])
            tm_proj(0, 512, 512)
            P.op("act", lambda e: e.activation(out=ktm[:].rearrange("p h d -> p (h d)"), in_=PS[:, 0, :], func=AF.Copy, scale=KSCALE), r=[("ps", 0)], w=["ktm"])
            tm_proj(1, 1024, 512)
            P.op("act", lambda e: e.copy(v[:, 0:2, :].rearrange("p h d -> p (h d)"), PS[:, 1, :]), r=[("ps", 1)], w=["v"])
            tm_proj(0, 1536, 512)
            P.op("act", lambda e: e.copy(v[:, 2:4, :].rearrange("p h d -> p (h d)"), PS[:, 0, :]), r=[("ps", 0)], w=["v"])
            if own:
                tm_proj(1, 2048, 512)
                P.op("act", lambda e: e.activation(out=og[:, 0:512], in_=PS[:, 1, :], func=AF.Sigmoid), r=[("ps", 1)], w=["og"])
                tm_proj(0, 2560, 512)
                P.op("act", lambda e: e.activation(out=og[:, 512:1024], in_=PS[:, 0, :], func=AF.Sigmoid), r=[("ps", 0)], w=["og"])
            tm_proj(3, 3072, 8)
            P.op("dve", lambda e: e.tensor_tensor(g[:, 0:2, :].rearrange("p a h -> p (a h)"), PS[:, 3, 0:8], bg[:], ALU.add), r=[("ps", 3), "bg"], w=["g01"])
            P.op("act", lambda e: e.activation(out=g[:, 5, :], in_=g[:, 1, :], func=AF.Exp, scale=-1.0), r=["g01"], w=["g5"])
            P.op("act", lambda e: e.activation(out=g[:, 6, :], in_=g[:, 5, :], func=AF.Ln, bias=ones[:, 0:1], scale=1.0), r=["g5", "CN"], w=["g6"])
            P.op("dve", lambda e: e.tensor_scalar(g[:, 1, :], g[:, 6, :], -1.0, None, ALU.mult), r=["g6", "g01"], w=["g01"])
            P.op("pe", lambda e: e.matmul(PS[:, 3, 8:12], Tri, g[:, 1, :], start=True, stop=True), r=["CN", "g01"], w=[("ps", 3)])
            P.op("dve", lambda e: e.tensor_tensor(g[:, 2, :], PS[:, 3, 8:12], car[:, 0, :], ALU.add), r=[("ps", 3), "car"], w=["g2"])
            P.op("dve", lambda e: e.tensor_tensor(g[:, 3, :], g[:, 0, :], g[:, 2, :], ALU.subtract), r=["g01", "g2"], w=["g3"])
            P.op("pe", lambda e: e.matmul(PS[:, 3, 12:16], ones, g[:, 1, :], start=True, stop=True), r=["CN", "g01"], w=[("ps", 3)])
            P.op("dve", lambda e: e.tensor_tensor(car[:, 0, :], PS[:, 3, 12:16], car[:, 0, :], ALU.add), r=[("ps", 3), "car", "g2"], w=["car"])
            for hd in range(4):
                P.op("dve", lambda e, hd=hd: e.tensor_scalar(abc[:, hd, :], ones, g[:, 3, hd:hd + 1], None, ALU.mult), r=["CN", "g3"], w=["abc"])
            parep = PS[:, 3, :].rearrange("p (h t) -> p h t", t=128)
            for hd in range(4):
                P.op("pe", lambda e, hd=hd: e.matmul(parep[:, hd, :], abc[:, hd, :], I4[:, 0, :], start=True, stop=True), r=["abc", "CN"], w=[("ps", 3)])
            P.op("act", lambda e: e.copy(arep[:], parep), r=[("ps", 3)], w=["arep"])
            P.op("dve", lambda e: e.tensor_copy(s8[:, 0, :], car[:, 1, :]), r=["car"], w=["s80"])
            for hd in range(4):
                P.op("dve", lambda e, hd=hd: e.tensor_tensor_scan(out=Mrep[:, hd, :], data0=arep[:, hd, :], data1=arep[:, hd, :], initial=s8[:, 0, hd:hd + 1], op0=ALU.max, op1=ALU.max),
                     r=["arep", "s80"], w=["Mrep"])
            P.op("dve", lambda e: e.tensor_copy(car[:, 1, :], Mrep[:, :, 127]), r=["Mrep", "s80"], w=["car"])
            P.op("dve", lambda e: e.tensor_tensor(s8[:, 1, :], g[:, 3, :], car[:, 1, :], ALU.subtract), r=["g3", "car"], w=["s81"])
            P.op("dve", lambda e: e.tensor_tensor(s8[:, 2, :], s8[:, 0, :], car[:, 1, :], ALU.subtract), r=["s80", "car"], w=["s82"])
            P.op("act", lambda e: e.activation(out=s8[:, 3:5, :], in_=s8[:, 1:3, :], func=AF.Exp), r=["s81", "s82"], w=["s834"])
            if own:
                pq = PS[:, 2, :].rearrange("p (h t) -> p h t", t=128)
                for (c0, dst, scl, nm) in ((0, qT, 1.0, "qT"), (512, kT, KSCALE, "kT")):
                    for hd in range(4):
                        for k in range(8):
                            P.op("pe", lambda e, hd=hd, k=k, c0=c0: e.matmul(pq[:, hd, :], W[:, k, c0 + hd * 128:c0 + (hd + 1) * 128], xTc[:, k, :], start=(k == 0), stop=(k == 7)),
                                 r=["xTc", ("W", k)], w=[("ps", 2)])
                    P.op("act", lambda e, dst=dst, scl=scl: e.activation(out=dst[:], in_=pq, func=AF.Copy, scale=scl), r=[("ps", 2)], w=[nm])
                P.op("dve", lambda e: e.tensor_tensor(WT[:], Mrep[:], I4, ALU.mult), r=["Mrep", "CN"], w=["WT"])
                P.op("dve", lambda e: e.tensor_reduce(out=g[:, 4, :], in_=WT[:], axis=AX.X, op=ALU.add), r=["WT"], w=["g4"])
                P.op("dve", lambda e: e.tensor_tensor(g[:, 7, :], g[:, 2, :], g[:, 4, :], ALU.add), r=["g2", "g4"], w=["g7"])
                P.op("act", lambda e: e.activation(out=s8[:, 5, :], in_=g[:, 7, :], func=AF.Exp, scale=-1.0), r=["g7"], w=["s85"])
                for hd in range(4):
                    P.op("dve", lambda e, hd=hd: e.tensor_scalar(WT[:, hd, :], Mrep[:, hd, :], g[:, 3, hd:hd + 1], 0.0, ALU.subtract, ALU.max), r=["Mrep", "g3", "g4"], w=["WT"])
                P.op("act", lambda e: e.activation(out=WT[:], in_=WT[:], func=AF.Exp, scale=-1.0), r=["WT"], w=["WT"])
                P.op("dve", lambda e: e.tensor_tensor(WT[:], WT[:], Tri4, ALU.mult), r=["WT", "CN"], w=["WT"])
                for hd in range(4):
                    P.op("pe", lambda e, hd=hd: e.matmul(pq[:, hd, :], kT[:, hd, :], qT[:, hd, :], start=True, stop=True), r=["kT", "qT"], w=[("ps", 2)])
                P.op("dve", lambda e: e.tensor_tensor(AT[:], WT[:], pq, ALU.mult), r=["WT", ("ps", 2)], w=["AT"])
                for hd in range(4):
                    P.op("act", lambda e, hd=hd: e.activation(out=qp[:, hd, :], in_=Mrep[:, hd, :], func=AF.Exp, bias=s8[:, 0, hd:hd + 1], scale=-1.0), r=["Mrep", "s80"], w=["qp"])
                P.op("dve", lambda e: e.tensor_tensor(qp[:], qp[:], qT[:], ALU.mult), r=["qp", "qT"], w=["qp"])
                for hd in range(4):
                    po = PS[:, hd // 2, (hd % 2) * 256:(hd % 2 + 1) * 256]
                    P.op("pe", lambda e, hd=hd, po=po: e.matmul(po, qp[:, hd, :], Cst[:, hd, :], start=True, stop=False), r=["qp", "C"], w=[("ps", hd // 2)])
                    P.op("pe", lambda e, hd=hd, po=po: e.matmul(po, AT[:, hd, :], v[:, hd, :], start=False, stop=True), r=["AT", "v"], w=[("ps", hd // 2)])
                for hd in range(4):
                    pd = PS[:, 3, 16 + hd:17 + hd]
                    P.op("pe", lambda e, hd=hd, pd=pd: e.matmul(pd, qp[:, hd, :], nst[:, hd:hd + 1], start=True, stop=False), r=["qp", "n"], w=[("ps", 3)])
                    P.op("pe", lambda e, hd=hd, pd=pd: e.matmul(pd, AT[:, hd, :], ones[:, 0:1], start=False, stop=True), r=["AT", "CN"], w=[("ps", 3)])
                P.op("dve", lambda e: e.tensor_scalar(s8[:, 6, :], PS[:, 3, 16:20], -1.0, None, ALU.mult), r=[("ps", 3)], w=["s86"])
                P.op("dve", lambda e: e.tensor_tensor(s8[:, 6, :], s8[:, 6, :], PS[:, 3, 16:20], ALU.max), r=[("ps", 3), "s86"], w=["s86"])
                P.op("dve", lambda e: e.tensor_tensor(s8[:, 6, :], s8[:, 6, :], s8[:, 5, :], ALU.max), r=["s86", "s85"], w=["s86"])
                P.op("dve", lambda e: e.reciprocal(s8[:, 7, :], s8[:, 6, :]), r=["s86"], w=["s87"])
                for hd in range(4):
                    po = PS[:, hd // 2, (hd % 2) * 256:(hd % 2 + 1) * 256]
                    P.op("dve", lambda e, hd=hd, po=po: e.tensor_scalar(hh[:, hd, :], po, s8[:, 7, hd:hd + 1], None, ALU.mult), r=[("ps", hd // 2), "s87"], w=["hh"])
                P.op("dve", lambda e: e.tensor_reduce(out=g[:, 5, :], in_=hh[:], axis=AX.X, op=ALU.add), r=["hh", "g5"], w=["g5"])
                P.op("act", lambda e: e.activation(out=hsq[:], in_=hh[:], func=AF.Square), r=["hh"], w=["hsq"])
                P.op("dve", lambda e: e.tensor_reduce(out=g[:, 6, :], in_=hsq[:], axis=AX.X, op=ALU.add), r=["hsq", "g6"], w=["g6"])
                P.op("dve", lambda e: e.tensor_scalar(g[:, 5:7, :], g[:, 5:7, :], 1.0 / 256, None, ALU.mult), r=["g5", "g6"], w=["g5", "g6"])
                P.op("dve", lambda e: e.tensor_tensor(s8[:, 1, :], g[:, 5, :], g[:, 5, :], ALU.mult), r=["g5", "s81"], w=["s81"])
                P.op("dve", lambda e: e.tensor_tensor(s8[:, 1, :], g[:, 6, :], s8[:, 1, :], ALU.subtract), r=["g6", "s81"], w=["s81"])
                P.op("dve", lambda e: e.tensor_scalar(s8[:, 1, :], s8[:, 1, :], LN_EPS, None, ALU.add), r=["s81"], w=["s81"])
                P.op("act", lambda e: e.sqrt(s8[:, 2, :], s8[:, 1, :]), r=["s81", "s82"], w=["s82"])
                P.op("dve", lambda e: e.reciprocal(s8[:, 1, :], s8[:, 2, :]), r=["s82"], w=["s81"])
                for hd in range(4):
                    P.op("dve", lambda e, hd=hd: e.tensor_scalar(hh[:, hd, :], hh[:, hd, :], g[:, 5, hd:hd + 1], s8[:, 1, hd:hd + 1], ALU.subtract, ALU.mult), r=["hh", "g5", "s81"], w=["hh"])
                hflat = hh[:].rearrange("p h d -> p (h d)")
                P.op("dve", lambda e: e.tensor_tensor(hflat, hflat, ng[:], ALU.mult), r=["hh", "ng"], w=["hh"])
                P.op("dve", lambda e: e.tensor_tensor(hg[:], hflat, og[:], ALU.mult), r=["hh", "og"], w=["hg"])
                for k in range(8):
                    P.op("pe", lambda e, k=k: e.transpose(PSb[:, k * 128:(k + 1) * 128], hg[:, k * 128:(k + 1) * 128], identb[:]), r=["hg", "identb"], w=[("ps", 2)])
                P.op("act", lambda e: e.copy(hgT[:], PSb[:].rearrange("p (c n) -> p c n", n=128)), r=[("ps", 2)], w=["hgT"])
                for hf in range(2):
                    for k in range(8):
                        P.op("pe", lambda e, hf=hf, k=k: e.matmul(PS[:, hf, :], hgT[:, k, :], Wo[:, k, hf * 512:(hf + 1) * 512], start=(k == 0), stop=(k == 7)), r=["hgT", ("Wo", k)], w=[("ps", hf)])
                P.op("dve", lambda e, co=co: e.scalar_tensor_tensor(out=X[:, co, :], in0=X[:, co, :], scalar=ALPHA, in1=PS[:, 0:2, :].rearrange("p b n -> p (b n)"), op0=ALU.mult, op1=ALU.add),
                     r=[("X", co), ("ps", 0), ("ps", 1)], w=[("X", co)])
            for hd in range(4):
                P.op("dve", lambda e, hd=hd: e.tensor_scalar(ks[:, hd, :], ktm[:, hd, :], s8[:, 3, hd:hd + 1], None, ALU.mult), r=["ktm", "s834"], w=["ks"])
            for hd in range(4):
                po = PS[:, hd // 2, (hd % 2) * 256:(hd % 2 + 1) * 256]
                P.op("pe", lambda e, hd=hd, po=po: e.matmul(po, ks[:, hd, :], v[:, hd, :], start=True, stop=True), r=["ks", "v"], w=[("ps", hd // 2)])
            for hd in range(4):
                P.op("pe", lambda e, hd=hd: e.matmul(PS[:, 3, 24 + hd:25 + hd], ks[:, hd, :], ones[:, 0:1], start=True, stop=True), r=["ks", "CN"], w=[("ps", 3)])
            for hd in range(4):
                po = PS[:, hd // 2, (hd % 2) * 256:(hd % 2 + 1) * 256]
                P.op("dve", lambda e, hd=hd, po=po: e.scalar_tensor_tensor(out=Cst[:, hd, :], in0=Cst[:, hd, :], scalar=s8[:, 4, hd:hd + 1], in1=po, op0=ALU.mult, op1=ALU.add),
                     r=["C", "s834", ("ps", hd // 2)], w=["C"])
            P.op("dve", lambda e: e.tensor_tensor(nst[:], nst[:], s8[:, 4, :], ALU.mult), r=["n", "s834"], w=["n"])
            P.op("dve", lambda e: e.tensor_tensor(nst[:], nst[:], PS[:, 3, 24:28], ALU.add), r=["n", ("ps", 3)], w=["n"])
            if c == NTP - 1:
                P.op("dve", lambda e: e.tensor_scalar(Cst[:].rearrange("p h d -> p (h d)"), Cst[:].rearrange("p h d -> p (h d)"), flg[:, 0:1], None, ALU.mult), r=["C", "flag"], w=["C"])
                P.op("dve", lambda e: e.tensor_scalar(nst[:], nst[:], flg[:, 0:1], None, ALU.mult), r=["n", "flag"], w=["n"])
                P.op("dve", lambda e: e.tensor_scalar(car[:].rearrange("p a h -> p (a h)"), car[:].rearrange("p a h -> p (a h)"), flg[:, 0:1], None, ALU.mult), r=["car", "flag"], w=["car"])
        P.barrier(); P.emit()
    if lng is not None:
        with ExitStack() as sc:
            layer_norm_out(P, nc, sc, X, lng, lnb, out_dram, "lnm")
            P.barrier(); P.emit()


def mlstm_consts():
    import numpy as np
    cn = np.zeros((128, 1280), np.float32)
    tri = (np.arange(128)[:, None] <= np.arange(128)[None, :]).astype(np.float32)
    eye = np.eye(128, dtype=np.float32)
    cn[:, 0:128] = tri
    for h in range(4):
        cn[:, 128 + h * 128:128 + (h + 1) * 128] = eye
        cn[:, 640 + h * 128:640 + (h + 1) * 128] = tri
    cn[:, 1152:1280] = 1.0
    return cn


def build_mlstm_prog(NTK=32, NTO=16, ln=True):
    nc = bass.Bass("TRN2", target_bir_lowering=False)
    dt = lambda name, shape, kind="ExternalInput", d=F32: nc.dram_tensor(name, shape, d, kind=kind).ap()
    xkv = dt("xkv", [NTK * 128, 1024]); identd = dt("ident", [128, 128])
    w_in = dt("w_in", [1024, 3080]); b_gates = dt("b_gates", [1, 8]); norm_g = dt("norm_g", [1, 1024]); w_out = dt("w_out", [1024, 1024])
    consts = dt("consts", [128, 1280]); flag = dt("flag", [128, 1])
    lng = dt("lng", [1, 1024]); lnb = dt("lnb", [1, 1024])
    y = dt("y", [NTO * 128, 1024], "ExternalOutput")
    with ExitStack() as st:
        P = Prog(nc, st)
        X = sbt(nc, st, "X", [128, NTO, 1024], F32)
        ident = sbt(nc, st, "identsb", [128, 128], F32)
        PS = st.enter_context(nc.psum_tensor("PS", [128, 8, 512], F32))
        P.op("sp", lambda e: e.dma_start(out=ident[:], in_=identd), w=["ident"], dma=True)
        mlstm_stage(P, nc, X, PS, ident, xkv, w_in, b_gates, norm_g, w_out, consts, flag, lng if ln else None, lnb, y, NTK=NTK, NTO=NTO)
        print("total ops", P.n_total)
    return nc
from contextlib import ExitStack


def build_fused_prog(E=32, NH=16, NTO=16, groups=None):
    NTK = 2 * NTO; TO = NTO * 128; TK = NTK * 128
    if groups is None:
        groups = [[0, 1], [2, 3], [4, 5], [6, 7]]
    nc = bass.Bass("TRN2", target_bir_lowering=False)
    dt = lambda name, shape, kind="ExternalInput", d=F32: nc.dram_tensor(name, shape, d, kind=kind).ap()
    xkv = dt("xkv", [TK, 1024]); pos = dt("pos", [1, TK], d=I32); identd = dt("ident", [128, 128])
    wqkv = dt("wqkv", [1024, 3072]); wo = dt("wo", [1024, 1024])
    ropec = dt("ropec", [128, 8]); pastmask = dt("pastmask", [128, NTO * (NTK // 2)]); causal = dt("causal", [128, 512])
    w_in = dt("w_in", [1024, 3080]); b_gates = dt("b_gates", [1, 8]); norm_g = dt("norm_g", [1, 1024]); w_out = dt("w_out", [1024, 1024])
    consts = dt("consts", [128, 1280]); flag = dt("flag", [128, 1])
    lnmg = [dt("lnmg%d" % L, [1, 1024]) for L in range(2)]; lnmb = [dt("lnmb%d" % L, [1, 1024]) for L in range(2)]
    lnfg = [dt("lnfg%d" % L, [1, 1024]) for L in range(2)]; lnfb = [dt("lnfb%d" % L, [1, 1024]) for L in range(2)]
    rw = [dt("rw%d" % L, [1024, E]) for L in range(2)]; rb = [dt("rb%d" % L, [1, E]) for L in range(2)]
    wgu = [dt("wgu%d" % L, [E, 1024, 2048]) for L in range(2)]; bgu = [dt("bgu%d" % L, [E, 2048]) for L in range(2)]
    wd = [dt("wd%d" % L, [E, 1024, 1024]) for L in range(2)]; bd = [dt("bd%d" % L, [E, 1024]) for L in range(2)]
    y = dt("y", [TO, 1024], "ExternalOutput")
    xch_in = nc.dram_tensor("xch_in", [TO // 512, 512, 1024], F32)
    xch_out = nc.dram_tensor("xch_out", [TO // 512, 1024, 1024], F32)
    with ExitStack() as st:
        P = Prog(nc, st)
        ident = sbt(nc, st, "identsb", [128, 128], F32)
        PS = st.enter_context(nc.psum_tensor("PS", [128, 8, 512], F32))
        P.op("sp", lambda e: e.dma_start(out=ident[:], in_=identd), w=["ident"], dma=True)
        X = attn_stage(P, nc, None, PS, ident, xkv, pos, wqkv, wo, ropec, pastmask, causal, lnmg[0], lnmb[0], None, NTK=NTK, NTO=NTO, NH=NH, xstack=st)
        moe_stage(P, nc, X, PS, ident, rw[0], rb[0], wgu[0], bgu[0], wd[0], bd[0], lnfg[0], lnfb[0], None, E=E)
        NCH = TO // 512
        for tt in range(NTO):
            j, r = divmod(tt, 4)
            P.op("sp", lambda e, tt=tt, j=j, r=r: e.dma_start(out=xch_in.ap()[j, r * 128:(r + 1) * 128, :], in_=X[:, tt, :]), r=[("X", tt)], w=[("xin", tt)], dma=True, lane=P.rr_lane("xo", 4))
        for j in range(NCH):
            P.op("pool", lambda e, j=j: e.collective_compute("AllGather", ALU.bypass, replica_groups=groups, ins=[xch_in.ap()[j].opt()], outs=[xch_out.ap()[j].opt()]),
                 r=[("xin", tt) for tt in range(4 * j, 4 * j + 4)], w=[("xout", j)], dma=True, lane="cc", inc=1)
        P.barrier(); P.emit()
        mlstm_stage(P, nc, X, PS, ident, None, w_in, b_gates, norm_g, w_out, consts, flag, lnmg[1], lnmb[1], None, NTK=NTK, NTO=NTO, x_in_sbuf=True, xkv_keys=[("xout", j) for j in range(TO // 512)], prev_src=lambda c: xch_out.ap()[c // 4, (c % 4) * 128:(c % 4 + 1) * 128, :])
        moe_stage(P, nc, X, PS, ident, rw[1], rb[1], wgu[1], bgu[1], wd[1], bd[1], lnfg[1], lnfb[1], y, E=E)
        print("total ops", P.n_total)
    return nc

_PROGS = {}


def kernel(x, positions, attn_w_qkv, attn_w_o, mlstm_w_in, mlstm_b_gates, mlstm_norm_g, mlstm_w_out,
           ln_mix_g, ln_mix_b, ln_ffn_g, ln_ffn_b, router_w, router_b, w_gate_up, b_gate_up, w_down, b_down):
    f32 = lambda a: np.ascontiguousarray(np.asarray(a, dtype=np.float32))
    x = f32(x)
    positions = np.ascontiguousarray(np.asarray(positions, dtype=np.int32))
    ident = np.eye(128, dtype=np.float32)
    H = 2048
    row = lambda a: f32(a).reshape(1, -1)
    if "fused" not in _PROGS:
        _PROGS["fused"] = build_fused_prog()
    nc = _PROGS["fused"]
    rc = rope_consts()
    cn = mlstm_consts()
    shared = dict(ident=ident, wqkv=f32(attn_w_qkv[0]), wo=f32(attn_w_o[0]), ropec=rc,
                  w_in=f32(mlstm_w_in[0]), b_gates=row(mlstm_b_gates[0]), norm_g=row(mlstm_norm_g[0]), w_out=f32(mlstm_w_out[0]), consts=cn)
    for L in range(2):
        shared.update({"lnmg%d" % L: row(ln_mix_g[L]), "lnmb%d" % L: row(ln_mix_b[L]), "lnfg%d" % L: row(ln_ffn_g[L]), "lnfb%d" % L: row(ln_ffn_b[L]),
                       "rw%d" % L: f32(router_w[L]), "rb%d" % L: row(router_b[L]), "wgu%d" % L: f32(w_gate_up[L]), "bgu%d" % L: f32(b_gate_up[L]),
                       "wd%d" % L: f32(w_down[L]), "bd%d" % L: f32(b_down[L])})
    maps = []
    for c in range(8):
        b, h = divmod(c, 2)
        if h == 1:
            xkv = x[b]; pos = positions[b]
        else:
            xkv = np.concatenate([x[b, :H], x[b, :H]], 0); pos = np.concatenate([positions[b, :H], positions[b, :H]], 0)
        pm, cm = attn_masks(32, 16, h == 1)
        d = dict(shared)
        d.update(xkv=np.ascontiguousarray(xkv), pos=np.ascontiguousarray(pos).reshape(1, -1), pastmask=pm, causal=cm,
                 flag=np.full((128, 1), float(h), np.float32))
        maps.append(d)
    res = run_bass_kernel_spmd(nc, maps, core_ids=list(range(8)))
    ys = [r["y"] for r in res.results]
    out = np.stack([np.concatenate([ys[2 * b], ys[2 * b + 1]], 0) for b in range(4)], 0)
    return out.astype(np.float32)
```
